# Optimizing a Trainium2 kernel written in Bass

```python
import math
import jax, jax.numpy as jnp
from jax import lax
import numpy as np

D_MODEL = 2048
BATCH = 1
SEQ = 16384
DEPTH = 2

D_MIX = D_MODEL
S5_WIDTH = D_MIX // 4
S5_CH = 16
S5_GROUPS = S5_WIDTH // S5_CH
S5_STATE = 64
S5_DT_MIN = 0.001
S5_DT_MAX = 0.1
MLA_HEADS = 8
MLA_NOPE = 128
MLA_ROPE = 64
MLA_V = 128
MLA_Q_RANK = 512
MLA_KV_RANK = 256
MLA_WIDTH = MLA_HEADS * MLA_V
MLA_BLOCK = 128
ROPE_BASE = 10000.0
HG_WIDTH = D_MIX - S5_WIDTH - MLA_WIDTH
HG_HEAD_DIM = 128
HG_HEADS = HG_WIDTH // HG_HEAD_DIM
HG_CHUNK = 64
IN_COLS = S5_WIDTH + MLA_Q_RANK + MLA_KV_RANK + MLA_ROPE + 4 * HG_WIDTH
D_FF = 5632
N_EXPERTS = 8
TOP_K = 2
D_FF_EXPERT = 7168
N_DENSE = (DEPTH + 1) // 2
N_MOE = DEPTH // 2
DEEPNORM_ALPHA = (2 * DEPTH) ** 0.25
DEEPNORM_BETA = (8 * DEPTH) ** -0.25
LN_EPS = 1e-5
RMS_EPS = 1e-6

kernel_name = 'hybrid_s5_mla_hgrn2_deepnorm_moe'

F32 = jnp.float32


def layer_norm(x, g, b):
    xf = x.astype(F32)
    mu = jnp.mean(xf, axis=-1, keepdims=True)
    xc = xf - mu
    var = jnp.mean(xc * xc, axis=-1, keepdims=True)
    return (xc * lax.rsqrt(var + LN_EPS) * g.astype(F32) + b.astype(F32)).astype(x.dtype)


def rms_norm(x, g):
    xf = x.astype(F32)
    ms = jnp.mean(xf * xf, axis=-1, keepdims=True)
    return (xf * lax.rsqrt(ms + RMS_EPS) * g.astype(F32)).astype(x.dtype)


def rope(x, cos, sin):
    half = x.shape[-1] // 2
    x1, x2 = x[..., :half], x[..., half:]
    return jnp.concatenate([x1 * cos - x2 * sin, x1 * sin + x2 * cos], axis=-1).astype(x.dtype)


def s5_mixer(u, a_re, a_im, b_re, b_im, c_re, c_im, d, log_dt, w_glu, b_glu):
    bsz, seq, _ = u.shape
    uf = u.astype(F32).reshape(bsz, seq, S5_GROUPS, S5_CH)
    lam = lax.complex(jnp.minimum(a_re.astype(F32), -1e-4), a_im.astype(F32))
    dt = jnp.exp(log_dt.astype(F32))[:, None]
    lam_bar = jnp.exp(lam * dt)
    b = lax.complex(b_re.astype(F32), b_im.astype(F32))
    b_bar = ((lam_bar - 1.0) / lam)[..., None] * b
    bu = jnp.einsum('blgc,gpc->blgp', uf.astype(jnp.complex64), b_bar)
    a = jnp.broadcast_to(lam_bar, bu.shape)

    def combine(left, right):
        a_l, s_l = left
        a_r, s_r = right
        return a_r * a_l, a_r * s_l + s_r

    _, states = lax.associative_scan(combine, (a, bu), axis=1)
    c = lax.complex(c_re.astype(F32), c_im.astype(F32))
    y = jnp.real(jnp.einsum('gcp,blgp->blgc', c, states)) + d.astype(F32).reshape(S5_GROUPS, S5_CH) * uf
    y = jax.nn.gelu(y.reshape(bsz, seq, S5_WIDTH))
    y = y * jax.nn.sigmoid(y @ w_glu.astype(F32) + b_glu.astype(F32))
    return y.astype(u.dtype)


def mla_mixer(cq, ckv, kr, q_norm, w_uq, kv_norm, w_ukv, cos, sin):
    bsz, seq, _ = cq.shape
    q = jnp.einsum('blr,rhd->blhd', rms_norm(cq, q_norm), w_uq)
    q = jnp.concatenate([q[..., :MLA_NOPE], rope(q[..., MLA_NOPE:], cos[:, None], sin[:, None])], axis=-1)
    kv = jnp.einsum('blr,rhd->blhd', rms_norm(ckv, kv_norm), w_ukv)
    k_rope = rope(kr, cos, sin)
    k = jnp.concatenate([kv[..., :MLA_NOPE],
                         jnp.broadcast_to(k_rope[:, :, None, :], (bsz, seq, MLA_HEADS, MLA_ROPE))], axis=-1)
    v = kv[..., MLA_NOPE:]
    scale = (MLA_NOPE + MLA_ROPE) ** -0.5
    n_blocks = seq // MLA_BLOCK
    q_blocks = q.reshape(bsz, n_blocks, MLA_BLOCK, MLA_HEADS, MLA_NOPE + MLA_ROPE).transpose(1, 0, 2, 3, 4)
    key_pos = jnp.arange(seq)

    def attend(args):
        q_blk, blk = args
        s = jnp.einsum('bqhd,bkhd->bhqk', q_blk, k).astype(F32) * scale
        q_pos = blk * MLA_BLOCK + jnp.arange(MLA_BLOCK)
        s = jnp.where(key_pos[None, :] <= q_pos[:, None], s, -jnp.inf)
        p = jax.nn.softmax(s, axis=-1).astype(v.dtype)
        return jnp.einsum('bhqk,bkhd->bqhd', p, v)

    out = lax.map(attend, (q_blocks, jnp.arange(n_blocks)))
    return out.transpose(1, 0, 2, 3, 4).reshape(bsz, seq, MLA_WIDTH)


def hgrn2_mixer(q_raw, f_raw, i_raw, g_raw, lb, o_norm):
    bsz, seq, _ = q_raw.shape

    def heads(t):
        return t.astype(F32).reshape(bsz, seq, HG_HEADS, HG_HEAD_DIM)

    q = jax.nn.silu(heads(q_raw))
    z = heads(f_raw)
    lb_h = lb.astype(F32).reshape(HG_HEADS, HG_HEAD_DIM)
    log_f = jnp.logaddexp(jnp.log(lb_h), jnp.log1p(-lb_h) + jax.nn.log_sigmoid(z))
    k = (1.0 - lb_h) * jax.nn.sigmoid(-z)
    v = heads(i_raw)
    n_chunks = seq // HG_CHUNK

    def to_chunks(t):
        return t.reshape(bsz, n_chunks, HG_CHUNK, HG_HEADS, HG_HEAD_DIM).transpose(1, 0, 3, 2, 4)

    causal = jnp.tril(jnp.ones((HG_CHUNK, HG_CHUNK), dtype=bool))

    def step(state, chunk):
        q_c, k_c, v_c, lf_c = chunk
        b = jnp.cumsum(lf_c, axis=-2)
        o_inter = jnp.einsum('bhtd,bhde->bhte', q_c * jnp.exp(b), state)
        diff = jnp.where(causal[:, :, None], b[..., :, None, :] - b[..., None, :, :], -jnp.inf)
        attn = jnp.einsum('bhtd,bhsd,bhtsd->bhts', q_c, k_c, jnp.exp(diff))
        o = o_inter + jnp.einsum('bhts,bhse->bhte', attn, v_c)
        b_last = b[..., -1:, :]
        new_state = jnp.exp(b_last[..., 0, :])[..., None] * state + \
            jnp.einsum('bhsd,bhse->bhde', k_c * jnp.exp(b_last - b), v_c)
        return new_state, o

    state0 = jnp.zeros((bsz, HG_HEADS, HG_HEAD_DIM, HG_HEAD_DIM), F32)
    _, o = lax.scan(step, state0, (to_chunks(q), to_chunks(k), to_chunks(v), to_chunks(log_f)))
    o = o.transpose(1, 0, 3, 2, 4).reshape(bsz, seq, HG_HEADS, HG_HEAD_DIM)
    o = rms_norm(o, o_norm) * jax.nn.silu(heads(g_raw))
    return o.reshape(bsz, seq, HG_WIDTH).astype(q_raw.dtype)


def hybrid_mixer(x, w_in, w_out, s5_a_re, s5_a_im, s5_b_re, s5_b_im, s5_c_re, s5_c_im, s5_d,
                 s5_log_dt, s5_w_glu, s5_b_glu, mla_q_norm, mla_w_uq, mla_kv_norm, mla_w_ukv,
                 hg_lb, hg_o_norm, cos, sin):
    proj = x @ w_in
    sizes = (S5_WIDTH, MLA_Q_RANK, MLA_KV_RANK, MLA_ROPE) + (HG_WIDTH,) * 4
    offsets = np.cumsum(sizes)[:-1].tolist()
    u, cq, ckv, kr, hq, hf, hi, hg = jnp.split(proj, offsets, axis=-1)
    y_s5 = s5_mixer(u, s5_a_re, s5_a_im, s5_b_re, s5_b_im, s5_c_re, s5_c_im, s5_d, s5_log_dt, s5_w_glu, s5_b_glu)
    y_mla = mla_mixer(cq, ckv, kr, mla_q_norm, mla_w_uq, mla_kv_norm, mla_w_ukv, cos, sin)
    y_hg = hgrn2_mixer(hq, hf, hi, hg, hg_lb, hg_o_norm)
    y = jnp.concatenate([y_s5.astype(x.dtype), y_mla.astype(x.dtype), y_hg.astype(x.dtype)], axis=-1)
    return y @ w_out


def swiglu(x, w_gate, w_up, w_down):
    return (jax.nn.silu(x @ w_gate) * (x @ w_up)) @ w_down


def moe_ffn(x, w_router, w_gate, w_up, w_down):
    bsz, seq, d = x.shape
    xt = x.reshape(bsz * seq, d)
    logits = (xt @ w_router).astype(F32)
    top_vals, top_idx = lax.top_k(logits, TOP_K)
    top_w = jax.nn.softmax(top_vals, axis=-1)
    gates = jnp.sum(jax.nn.one_hot(top_idx, N_EXPERTS, dtype=F32) * top_w[..., None], axis=1)
    out = jnp.zeros((bsz * seq, d), F32)
    for e in range(N_EXPERTS):
        h = jax.nn.silu(xt @ w_gate[e]) * (xt @ w_up[e])
        out = out + gates[:, e:e + 1] * (h @ w_down[e]).astype(F32)
    return out.reshape(bsz, seq, d).astype(x.dtype)


def setup_inputs(seed: int = 0) -> dict:
    key = jax.random.key(seed)
    ks = iter(jax.random.split(key, 40))

    def nrm(shape, scale):
        return jax.random.normal(next(ks), shape, F32) * scale

    L = DEPTH
    G, P, C = S5_GROUPS, S5_STATE, S5_CH
    x = nrm((BATCH, SEQ, D_MODEL), 1.0)
    w_in = nrm((L, D_MODEL, IN_COLS), D_MODEL ** -0.5)
    w_out = nrm((L, D_MIX, D_MODEL), D_MIX ** -0.5 * DEEPNORM_BETA)
    s5_a_re = -0.5 + nrm((L, G, P), 0.01)
    s5_a_im = math.pi * jnp.arange(P, dtype=F32)[None, None, :] + nrm((L, G, P), 0.01)
    s5_b_re = nrm((L, G, P, C), (2 * C) ** -0.5)
    s5_b_im = nrm((L, G, P, C), (2 * C) ** -0.5)
    s5_c_re = nrm((L, G, C, P), P ** -0.5)
    s5_c_im = nrm((L, G, C, P), P ** -0.5)
    s5_d = nrm((L, S5_WIDTH), 1.0)
    s5_log_dt = jax.random.uniform(next(ks), (L, G), F32, math.log(S5_DT_MIN), math.log(S5_DT_MAX))
    s5_w_glu = nrm((L, S5_WIDTH, S5_WIDTH), S5_WIDTH ** -0.5)
    s5_b_glu = nrm((L, S5_WIDTH), 0.01)
    mla_q_norm = 1.0 + nrm((L, MLA_Q_RANK), 0.01)
    mla_w_uq = nrm((L, MLA_Q_RANK, MLA_HEADS, MLA_NOPE + MLA_ROPE), MLA_Q_RANK ** -0.5)
    mla_kv_norm = 1.0 + nrm((L, MLA_KV_RANK), 0.01)
    mla_w_ukv = nrm((L, MLA_KV_RANK, MLA_HEADS, MLA_NOPE + MLA_V), MLA_KV_RANK ** -0.5)
    hg_lb_logits = nrm((L, HG_WIDTH), 1.0)
    hg_o_norm = 1.0 + nrm((L, HG_HEAD_DIM), 0.01)
    ln1_g = 1.0 + nrm((L, D_MODEL), 0.01)
    ln1_b = nrm((L, D_MODEL), 0.01)
    ln2_g = 1.0 + nrm((L, D_MODEL), 0.01)
    ln2_b = nrm((L, D_MODEL), 0.01)
    ffn_w_gate = nrm((N_DENSE, D_MODEL, D_FF), D_MODEL ** -0.5)
    ffn_w_up = nrm((N_DENSE, D_MODEL, D_FF), D_MODEL ** -0.5)
    ffn_w_down = nrm((N_DENSE, D_FF, D_MODEL), D_FF ** -0.5 * DEEPNORM_BETA)
    moe_router = nrm((N_MOE, D_MODEL, N_EXPERTS), D_MODEL ** -0.5)
    moe_w_gate = nrm((N_MOE, N_EXPERTS, D_MODEL, D_FF_EXPERT), D_MODEL ** -0.5)
    moe_w_up = nrm((N_MOE, N_EXPERTS, D_MODEL, D_FF_EXPERT), D_MODEL ** -0.5)
    moe_w_down = nrm((N_MOE, N_EXPERTS, D_FF_EXPERT, D_MODEL), D_FF_EXPERT ** -0.5 * DEEPNORM_BETA)
    return {'x': x, 'w_in': w_in, 'w_out': w_out,
            's5_a_re': s5_a_re, 's5_a_im': s5_a_im, 's5_b_re': s5_b_re, 's5_b_im': s5_b_im,
            's5_c_re': s5_c_re, 's5_c_im': s5_c_im, 's5_d': s5_d, 's5_log_dt': s5_log_dt,
            's5_w_glu': s5_w_glu, 's5_b_glu': s5_b_glu,
            'mla_q_norm': mla_q_norm, 'mla_w_uq': mla_w_uq, 'mla_kv_norm': mla_kv_norm, 'mla_w_ukv': mla_w_ukv,
            'hg_lb_logits': hg_lb_logits, 'hg_o_norm': hg_o_norm,
            'ln1_g': ln1_g, 'ln1_b': ln1_b, 'ln2_g': ln2_g, 'ln2_b': ln2_b,
            'ffn_w_gate': ffn_w_gate, 'ffn_w_up': ffn_w_up, 'ffn_w_down': ffn_w_down,
            'moe_router': moe_router, 'moe_w_gate': moe_w_gate, 'moe_w_up': moe_w_up, 'moe_w_down': moe_w_down}


def reference(x, w_in, w_out, s5_a_re, s5_a_im, s5_b_re, s5_b_im, s5_c_re, s5_c_im, s5_d, s5_log_dt,
              s5_w_glu, s5_b_glu, mla_q_norm, mla_w_uq, mla_kv_norm, mla_w_ukv, hg_lb_logits, hg_o_norm,
              ln1_g, ln1_b, ln2_g, ln2_b, ffn_w_gate, ffn_w_up, ffn_w_down,
              moe_router, moe_w_gate, moe_w_up, moe_w_down):
    seq = x.shape[1]
    pos = jnp.arange(seq, dtype=F32)
    inv_freq = ROPE_BASE ** (-jnp.arange(0, MLA_ROPE, 2, dtype=F32) / MLA_ROPE)
    ang = pos[:, None] * inv_freq[None, :]
    cos, sin = jnp.cos(ang), jnp.sin(ang)
    lb_all = jnp.cumsum(jax.nn.softmax(hg_lb_logits.astype(F32), axis=0), axis=0)
    lb_all = lb_all - lb_all[:1]
    for layer in range(DEPTH):
        h = hybrid_mixer(x, w_in[layer], w_out[layer], s5_a_re[layer], s5_a_im[layer], s5_b_re[layer],
                         s5_b_im[layer], s5_c_re[layer], s5_c_im[layer], s5_d[layer], s5_log_dt[layer],
                         s5_w_glu[layer], s5_b_glu[layer], mla_q_norm[layer], mla_w_uq[layer],
                         mla_kv_norm[layer], mla_w_ukv[layer], lb_all[layer], hg_o_norm[layer], cos, sin)
        x = layer_norm(DEEPNORM_ALPHA * x + h, ln1_g[layer], ln1_b[layer])
        j = layer // 2
        if layer % 2 == 0:
            f = swiglu(x, ffn_w_gate[j], ffn_w_up[j], ffn_w_down[j])
        else:
            f = moe_ffn(x, moe_router[j], moe_w_gate[j], moe_w_up[j], moe_w_down[j])
        x = layer_norm(DEEPNORM_ALPHA * x + f, ln2_g[layer], ln2_b[layer])
    return x
```

```python
import numpy as np
from contextlib import ExitStack
import concourse.bass as bass
import concourse.mybir as mybir
from concourse.bass_utils import run_bass_kernel_spmd

F32 = mybir.dt.float32
BF16 = mybir.dt.bfloat16
I32 = mybir.dt.int32
U8 = mybir.dt.uint8
AF = mybir.ActivationFunctionType
ALU = mybir.AluOpType
AX = mybir.AxisListType

NCORES = 8
SEQ = 16384
D = 2048
TOK = SEQ // NCORES
IN_COLS = 3392
D_FF = 5632
D_FFE = 7168
NEXP = 8
ALPHA = 4 ** 0.25
LN_EPS = 1e-5
RMS_EPS = 1e-6


class Buf:
    __slots__ = ("name", "w", "r")

    def __init__(self, name=""):
        self.name = name
        self.w = None
        self.r = {}


class Eng:
    def __init__(self, k, name, inst, is_dma_step=False):
        self.k = k
        self.name = name
        self.inst = inst
        self.sem = None
        self.semkey = None
        self.n = 0
        self.epoch = 0
        self.seen = {}
        self.new_sem()

    def new_sem(self):
        self.sem = self.k.new_sem(f"{self.name}_e{self.epoch}")
        self.semkey = f"{self.name}_e{self.epoch}"
        self.epoch += 1
        self.n = 0


class KB:
    ROT = 30000

    def __init__(self):
        self.nc = bass.Bass("TRN2", target_bir_lowering=False)
        self.es = ExitStack()
        self.nsem = 0
        self.E = {}
        for name in ("tensor", "vector", "scalar", "gpsimd", "sync"):
            self.E[name] = Eng(self, name, getattr(self.nc, name))
        self.dpool = {}
        self.ndma = {}
        self.uid = 0

    def new_sem(self, name):
        self.nsem += 1
        return self.es.enter_context(self.nc.semaphore(f"s{self.nsem}_{name}"))

    def sb(self, name, shape, dtype):
        self.uid += 1
        return self.es.enter_context(self.nc.sbuf_tensor(f"{name}_{self.uid}", list(shape), dtype))

    def ps(self, name, shape, dtype=F32):
        self.uid += 1
        return self.es.enter_context(self.nc.psum_tensor(f"{name}_{self.uid}", list(shape), dtype))

    def din(self, name, shape, dtype=F32):
        return self.nc.dram_tensor(name, list(shape), dtype, kind="ExternalInput").ap()

    def dout(self, name, shape, dtype=F32):
        return self.nc.dram_tensor(name, list(shape), dtype, kind="ExternalOutput").ap()

    def dscratch(self, name, shape, dtype=F32):
        return self.nc.dram_tensor(name, list(shape), dtype, kind="Internal").ap()

    def _deps(self, reads, writes):
        deps = {}

        def add(tok):
            if tok is None:
                return
            key, sem, val = tok
            if key not in deps or deps[key][1] < val:
                deps[key] = (sem, val)

        for b in reads:
            add(b.w)
        for b in writes:
            add(b.w)
            for key, (sem, val) in b.r.items():
                add((key, sem, val))
        return deps

    def _wait(self, E, deps, skip_key=None):
        for key, (sem, val) in deps.items():
            if key == skip_key:
                continue
            if E.seen.get(key, 0) < val:
                E.inst.wait_ge(sem, val)
                E.seen[key] = val

    def _mark(self, tok, reads, writes):
        key, sem, val = tok
        for b in writes:
            b.w = tok
            b.r = {}
        for b in reads:
            b.r[key] = (sem, val)

    def op(self, eng, fn, r=(), w=()):
        E = self.E[eng]
        if E.n >= self.ROT:
            E.new_sem()
        deps = self._deps(r, w)
        self._wait(E, deps, skip_key=E.semkey if eng == "tensor" else None)
        ins = fn(E.inst)
        E.n += 1
        ins.then_inc(E.sem, 1)
        tok = (E.semkey, E.sem, E.n)
        E.seen[E.semkey] = max(E.seen.get(E.semkey, 0), 0)
        self._mark(tok, r, w)
        return ins

    def dma(self, q, out, in_, r=(), w=(), **kw):
        E = self.E[q]
        K = 8
        if q not in self.dpool:
            self.dpool[q] = [self.new_sem(f"dma_{q}_{i}") for i in range(K)]
            self.ndma[q] = 0
        i = self.ndma[q]
        self.ndma[q] += 1
        sem = self.dpool[q][i % K]
        key = f"dma_{q}_{i % K}"
        prev = 16 * (i // K)
        deps = self._deps(r, w)
        if prev > 0:
            if key not in deps or deps[key][1] < prev:
                deps[key] = (sem, prev)
        self._wait(E, deps)
        ins = E.inst.dma_start(out=out, in_=in_, **kw)
        ins.then_inc(sem, 16)
        tok = (key, sem, prev + 16)
        self._mark(tok, r, w)
        return ins

    def finish(self):
        E = self.E["sync"]
        for q, pool in self.dpool.items():
            n = self.ndma[q]
            K = len(pool)
            for j, sem in enumerate(pool):
                cnt = (n - j + K - 1) // K if n > j else 0
                if cnt > 0:
                    key = f"dma_{q}_{j}"
                    if E.seen.get(key, 0) < 16 * cnt:
                        E.inst.wait_ge(sem, 16 * cnt)
                        E.seen[key] = 16 * cnt
        for name in ("tensor", "vector", "scalar", "gpsimd"):
            En = self.E[name]
            if En.n > 0 and E.seen.get(En.semkey, 0) < En.n:
                E.inst.wait_ge(En.sem, En.n)
        self.es.close()
        return self.nc


def bufs(n, name="b"):
    return [Buf(f"{name}{i}") for i in range(n)]


def build_A():
    k = KB()
    nc = k.nc
    xT = k.din("xT", [D, TOK])
    w = k.din("w", [D, IN_COLS])
    out = k.dout("projT", [IN_COLS, TOK])
    KC = D // 128
    wbf = k.sb("wbf", [128, KC, IN_COLS], BF16)
    wb = bufs(KC, "w")
    for kc in range(KC):
        k.dma("gpsimd", out=wbf[:, kc, :], in_=w[kc * 128:(kc + 1) * 128, :], w=[wb[kc]])
    NT = TOK // 512
    xbf = [k.sb("xbf", [128, KC, 512], BF16) for _ in range(2)]
    xb = bufs(2, "x")
    NPS = 4
    pst = [k.ps("ps", [128, 512]) for _ in range(NPS)]
    pb = bufs(NPS, "ps")
    NOB = 4
    obt = [k.sb("ob", [128, 512], F32) for _ in range(NOB)]
    ob = bufs(NOB, "ob")
    xTv = xT.rearrange("(kc p) t -> p kc t", p=128)
    mchunks = [(m0, min(128, IN_COLS - m0)) for m0 in range(0, IN_COLS, 128)]
    it = 0
    for tt in range(NT):
        xs = tt % 2
        k.dma("gpsimd", out=xbf[xs][:], in_=xTv[:, :, tt * 512:(tt + 1) * 512], w=[xb[xs]])
        for (m0, msz) in mchunks:
            pi = it % NPS
            oi = it % NOB
            for kc in range(KC):
                k.op("tensor", lambda e: e.matmul(pst[pi][0:msz, :], lhsT=wbf[:, kc, m0:m0 + msz],
                                                  rhs=xbf[xs][:, kc, :], start=(kc == 0), stop=(kc == KC - 1)),
                     r=[wb[kc], xb[xs]], w=[pb[pi]])
            if it % 2 == 0:
                k.op("scalar", lambda e: e.copy(out=obt[oi][0:msz, :], in_=pst[pi][0:msz, :]), r=[pb[pi]], w=[ob[oi]])
            else:
                k.op("vector", lambda e: e.tensor_copy(out=obt[oi][0:msz, :], in_=pst[pi][0:msz, :]), r=[pb[pi]], w=[ob[oi]])
            k.dma("sync", out=out[m0:m0 + msz, tt * 512:(tt + 1) * 512], in_=obt[oi][0:msz, :], r=[ob[oi]])
            it += 1
    return k.finish()


_CACHE = {}


def get_prog(name, builder):
    if name not in _CACHE:
        _CACHE[name] = builder()
    return _CACHE[name]


def run(nc, in_maps):
    res = run_bass_kernel_spmd(nc, in_maps, core_ids=list(range(NCORES)))
    return res.results


class Cols:
    def __init__(self, k, n, P=128, name="sc"):
        self.t = k.sb(name, [P, n], F32)
        self.n = n
        self.i = 0

    def new(self):
        i = self.i
        self.i += 1
        assert self.i <= self.n
        return self.t[:, i:i + 1], Buf(f"c{i}")


def build_S5():
    k = KB()
    NT = SEQ // 512
    u = k.din("u", [64, SEQ])
    d_are = k.din("are2", [128, 4])
    d_aim = k.din("aim2", [128, 4])
    d_ldt = k.din("ldt", [128, 4])
    d_BRI = k.din("BRI", [128, 64])
    d_BIR = k.din("BIR", [128, 64])
    d_CRI = k.din("CRI", [128, 64])
    d_d = k.din("dvec", [64, 1])
    d_sgn = k.din("sgn", [128, 1])
    d_id = k.din("ident", [128, 128])
    out = k.dout("ys5", [64, SEQ])

    def load(src, shape):
        t = k.sb("p", shape, F32)
        b = Buf()
        k.dma("sync", out=t[:], in_=src, w=[b])
        return t, b

    P_are, b_are = load(d_are, [128, 4])
    P_aim, b_aim = load(d_aim, [128, 4])
    P_ldt, b_ldt = load(d_ldt, [128, 4])
    P_BRI, b_BRI = load(d_BRI, [128, 64])
    P_BIR, b_BIR = load(d_BIR, [128, 64])
    P_CRI, b_CRI = load(d_CRI, [128, 64])
    P_d, b_d = load(d_d, [64, 1])
    P_sgn, b_sgn = load(d_sgn, [128, 1])
    P_id, b_id = load(d_id, [128, 128])

    cols = Cols(k, 400)

    def V(fn, r, w):
        return k.op("vector", fn, r=r, w=w)

    def mul(a, b):
        o = cols.new()
        V(lambda e: e.tensor_tensor(out=o[0], in0=a[0], in1=b[0], op=ALU.mult), [a[1], b[1]], [o[1]])
        return o

    def stt(a, s, b, op1):
        o = cols.new()
        V(lambda e: e.scalar_tensor_tensor(out=o[0], in0=a[0], scalar=s[0], in1=b[0], op0=ALU.mult, op1=op1),
          [a[1], s[1], b[1]], [o[1]])
        return o

    def act(a, func, scale=1.0, bias=0.0):
        o = cols.new()
        k.op("scalar", lambda e: e.activation(out=o[0], in_=a[0], func=func, scale=scale, bias=bias), r=[a[1]], w=[o[1]])
        return o

    def square_c(c, s):
        t = mul(s, s)
        s2 = cols.new()
        V(lambda e: e.tensor_scalar(out=s2[0], in0=s[0], scalar1=c[0], scalar2=2.0, op0=ALU.mult, op1=ALU.mult),
          [s[1], c[1]], [s2[1]])
        c2 = stt(c, c, t, ALU.subtract)
        return c2, s2

    nsgn = cols.new()
    V(lambda e: e.tensor_scalar(out=nsgn[0], in0=P_sgn[:, 0:1], scalar1=-1.0, scalar2=None, op0=ALU.mult), [b_sgn], [nsgn[1]])
    halfpi = cols.new()
    V(lambda e: e.memset(halfpi[0], float(np.pi / 2)), [], [halfpi[1]])

    PB = k.sb("PB", [128, 64], F32)
    bPB = Buf()
    pst = k.ps("pst", [64, 128])
    bpst = Buf()
    L1, L2, L3, COS, SIN, RR, W9 = [], [], [], [], [], [], []
    for j in range(4):
        are_raw = (P_are[:, j:j + 1], b_are)
        aim = (P_aim[:, j:j + 1], b_aim)
        ldt = (P_ldt[:, j:j + 1], b_ldt)
        are = cols.new()
        V(lambda e: e.tensor_scalar(out=are[0], in0=are_raw[0], scalar1=-1e-4, scalar2=None, op0=ALU.min), [b_are], [are[1]])
        dt = act(ldt, AF.Exp)
        ad = mul(are, dt)
        r = act(ad, AF.Exp)
        th = mul(aim, dt)
        s = act(th, AF.Sin, scale=1.0 / 64)
        c = cols.new()
        k.op("scalar", lambda e: e.activation(out=c[0], in_=th[0], func=AF.Sin, scale=1.0 / 64, bias=halfpi[0]),
             r=[th[1], halfpi[1]], w=[c[1]])
        for _ in range(6):
            c, s = square_c(c, s)
        Wk = [(c, s)]
        for _ in range(9):
            Wk.append(square_c(*Wk[-1]))
        W9.append(Wk[9])
        lbr = mul(r, c)
        lbi = mul(r, s)
        x = cols.new()
        V(lambda e: e.tensor_scalar(out=x[0], in0=lbr[0], scalar1=-1.0, scalar2=None, op0=ALU.add), [lbr[1]], [x[1]])
        den = stt(aim, aim, mul(are, are), ALU.add)
        rden = cols.new()
        V(lambda e: e.reciprocal(out=rden[0], in_=den[0]), [den[1]], [rden[1]])
        CRn = stt(x, are, mul(lbi, aim), ALU.add)
        CIn = stt(lbi, are, mul(x, aim), ALU.subtract)
        CR = mul(CRn, rden)
        CI = mul(CIn, rden)
        sCI = mul(CI, (P_sgn[:, 0:1], b_sgn))
        nsCR = mul(CR, nsgn)
        cs = slice(16 * j, 16 * j + 16)
        for which in range(2):
            V(lambda e: e.memset(PB[:], 0.0), [], [bPB])
            tmpc = k.sb("tmpc", [128, 16], F32)
            btmp = Buf()
            if which == 0:
                V(lambda e: e.tensor_scalar(out=tmpc[:], in0=P_BIR[:, cs], scalar1=sCI[0], scalar2=None, op0=ALU.mult),
                  [b_BIR, sCI[1]], [btmp])
                V(lambda e: e.scalar_tensor_tensor(out=PB[:, cs], in0=P_BRI[:, cs], scalar=CR[0], in1=tmpc[:],
                                                   op0=ALU.mult, op1=ALU.add), [b_BRI, CR[1], btmp], [bPB])
            else:
                V(lambda e: e.tensor_scalar(out=tmpc[:], in0=P_BRI[:, cs], scalar1=CI[0], scalar2=None, op0=ALU.mult),
                  [b_BRI, CI[1]], [btmp])
                V(lambda e: e.scalar_tensor_tensor(out=PB[:, cs], in0=P_BIR[:, cs], scalar=nsCR[0], in1=tmpc[:],
                                                   op0=ALU.mult, op1=ALU.add), [b_BIR, nsCR[1], btmp], [bPB])
            k.op("tensor", lambda e: e.transpose(pst[:], PB[:], P_id[:]), r=[bPB, b_id], w=[bpst])
            Lt = k.sb("L", [64, 128], BF16)
            bL = Buf()
            V(lambda e: e.tensor_copy(out=Lt[:], in_=pst[:]), [bpst], [bL])
            (L1 if which == 0 else L2).append((Lt, bL))
        L3t = k.sb("L3", [128, 64], BF16)
        bL3 = Buf()
        V(lambda e: e.memset(L3t[:], 0.0), [], [bL3])
        V(lambda e: e.tensor_scalar(out=L3t[:, cs], in0=P_CRI[:, cs], scalar1=nsgn[0], scalar2=None, op0=ALU.mult),
          [b_CRI, nsgn[1]], [bL3])
        L3.append((L3t, bL3))
        Ct = k.sb("COS", [128, 512], F32)
        St = k.sb("SIN", [128, 512], F32)
        bC, bS = Buf(), Buf()
        V(lambda e: e.memset(Ct[:, 0:1], 1.0), [], [bC])
        V(lambda e: e.memset(St[:, 0:1], 0.0), [], [bS])
        tmpt = k.sb("tmpt", [128, 256], F32)
        btt = Buf()
        for kk in range(9):
            n = 1 << kk
            ck, sk = Wk[kk]
            V(lambda e: e.tensor_scalar(out=tmpt[:, 0:n], in0=St[:, 0:n], scalar1=sk[0], scalar2=None, op0=ALU.mult),
              [bS, sk[1]], [btt])
            V(lambda e: e.scalar_tensor_tensor(out=Ct[:, n:2 * n], in0=Ct[:, 0:n], scalar=ck[0], in1=tmpt[:, 0:n],
                                               op0=ALU.mult, op1=ALU.subtract), [bC, ck[1], btt], [bC])
            V(lambda e: e.tensor_scalar(out=tmpt[:, 0:n], in0=Ct[:, 0:n], scalar1=sk[0], scalar2=None, op0=ALU.mult),
              [bC, sk[1]], [btt])
            V(lambda e: e.scalar_tensor_tensor(out=St[:, n:2 * n], in0=St[:, 0:n], scalar=ck[0], in1=tmpt[:, 0:n],
                                               op0=ALU.mult, op1=ALU.add), [bS, ck[1], btt], [bS])
        COS.append((Ct, bC))
        SIN.append((St, bS))
        Rt = k.sb("R", [128, 512], F32)
        bR = Buf()
        V(lambda e: e.memset(Rt[:], 1.0), [], [bR])
        V(lambda e: e.tensor_scalar(out=Rt[:], in0=Rt[:], scalar1=r[0], scalar2=None, op0=ALU.mult), [bR, r[1]], [bR])
        RR.append((Rt, bR))

    uf = [k.sb("uf", [64, 512], F32) for _ in range(2)]
    buf_uf = bufs(2)
    ubf = [k.sb("ubf", [64, 512], BF16) for _ in range(2)]
    buf_ubf = bufs(2)
    ps1 = [k.ps("ps1", [128, 512]) for _ in range(2)]
    ps2 = [k.ps("ps2", [128, 512]) for _ in range(2)]
    bps1, bps2 = bufs(2), bufs(2)
    psy = [k.ps("psy", [64, 512]) for _ in range(2)]
    bpsy = bufs(2)
    NTMP = 2
    tmp = {nm: [k.sb(nm, [128, 512], F32) for _ in range(NTMP)] for nm in ("ta", "tb", "tc", "td", "v1", "v2", "te", "tf")}
    btmp = {nm: bufs(NTMP) for nm in tmp}
    hb = [k.sb("h", [128, 512], BF16) for _ in range(NTMP)]
    bhb = bufs(NTMP)
    G1 = [[k.sb("g1", [128, 512], F32) for _ in range(2)] for _ in range(4)]
    G2 = [[k.sb("g2", [128, 512], F32) for _ in range(2)] for _ in range(4)]
    bG1 = [bufs(2) for _ in range(4)]
    bG2 = [bufs(2) for _ in range(4)]
    ycols = Cols(k, 4 * NT * 4 + 8, name="yc")
    yt = {nm: [k.sb(nm, [64, 512], F32) for _ in range(2)] for nm in ("y1", "y2", "y3", "y4", "yo")}
    byt = {nm: bufs(2) for nm in yt}
    it = 0
    for tt in range(NT):
        us = tt % 2
        k.dma("sync", out=uf[us][:], in_=u[:, tt * 512:(tt + 1) * 512], w=[buf_uf[us]])
        k.op("scalar", lambda e: e.copy(out=ubf[us][:], in_=uf[us][:]), r=[buf_uf[us]], w=[buf_ubf[us]])
        ys = tt % 2
        for j in range(4):
            pi = it % 2
            ti = it % NTMP
            it += 1
            Ct, bC = COS[j]
            St, bS = SIN[j]
            k.op("tensor", lambda e: e.matmul(ps1[pi][:], lhsT=L1[j][0][:], rhs=ubf[us][:], start=True, stop=True),
                 r=[L1[j][1], buf_ubf[us]], w=[bps1[pi]])
            k.op("tensor", lambda e: e.matmul(ps2[pi][:], lhsT=L2[j][0][:], rhs=ubf[us][:], start=True, stop=True),
                 r=[L2[j][1], buf_ubf[us]], w=[bps2[pi]])
            T = lambda nm: tmp[nm][ti]
            B = lambda nm: btmp[nm][ti]
            V(lambda e: e.tensor_tensor(out=T("ta")[:], in0=ps1[pi][:], in1=Ct[:], op=ALU.mult), [bps1[pi], bC], [B("ta")])
            V(lambda e: e.tensor_tensor(out=T("tb")[:], in0=ps2[pi][:], in1=St[:], op=ALU.mult), [bps2[pi], bS], [B("tb")])
            V(lambda e: e.tensor_tensor(out=T("tc")[:], in0=ps2[pi][:], in1=Ct[:], op=ALU.mult), [bps2[pi], bC], [B("tc")])
            V(lambda e: e.tensor_tensor(out=T("td")[:], in0=ps1[pi][:], in1=St[:], op=ALU.mult), [bps1[pi], bS], [B("td")])
            k.op("gpsimd", lambda e: e.tensor_tensor(out=T("v1")[:], in0=T("ta")[:], in1=T("tb")[:], op=ALU.add),
                 r=[B("ta"), B("tb")], w=[B("v1")])
            k.op("gpsimd", lambda e: e.tensor_tensor(out=T("v2")[:], in0=T("tc")[:], in1=T("td")[:], op=ALU.subtract),
                 r=[B("tc"), B("td")], w=[B("v2")])
            gs = tt % 2
            g1, g2 = G1[j][gs], G2[j][gs]
            if tt == 0:
                i1, i2 = 0.0, 0.0
                r1, r2 = [], []
            else:
                p1, p2 = G1[j][1 - gs], G2[j][1 - gs]
                bp1, bp2 = bG1[j][1 - gs], bG2[j][1 - gs]
                c9, s9 = W9[j]
                m1 = ycols.new()
                V(lambda e: e.tensor_scalar(out=m1[0], in0=p2[:, 511:512], scalar1=s9[0], scalar2=None, op0=ALU.mult),
                  [bp2, s9[1]], [m1[1]])
                a1 = ycols.new()
                V(lambda e: e.scalar_tensor_tensor(out=a1[0], in0=p1[:, 511:512], scalar=c9[0], in1=m1[0],
                                                   op0=ALU.mult, op1=ALU.subtract), [bp1, c9[1], m1[1]], [a1[1]])
                m2 = ycols.new()
                V(lambda e: e.tensor_scalar(out=m2[0], in0=p1[:, 511:512], scalar1=s9[0], scalar2=None, op0=ALU.mult),
                  [bp1, s9[1]], [m2[1]])
                a2 = ycols.new()
                V(lambda e: e.scalar_tensor_tensor(out=a2[0], in0=p2[:, 511:512], scalar=c9[0], in1=m2[0],
                                                   op0=ALU.mult, op1=ALU.add), [bp2, c9[1], m2[1]], [a2[1]])
                i1, i2 = a1[0], a2[0]
                r1, r2 = [a1[1]], [a2[1]]
            Rt, bR = RR[j]
            V(lambda e: e.tensor_tensor_scan(out=g1[:], data0=Rt[:], data1=T("v1")[:], initial=i1, op0=ALU.mult, op1=ALU.add),
              [bR, B("v1")] + r1, [bG1[j][gs]])
            V(lambda e: e.tensor_tensor_scan(out=g2[:], data0=Rt[:], data1=T("v2")[:], initial=i2, op0=ALU.mult, op1=ALU.add),
              [bR, B("v2")] + r2, [bG2[j][gs]])
            k.op("gpsimd", lambda e: e.tensor_tensor(out=T("te")[:], in0=g1[:], in1=Ct[:], op=ALU.mult),
                 r=[bG1[j][gs], bC], w=[B("te")])
            k.op("gpsimd", lambda e: e.tensor_tensor(out=T("tf")[:], in0=g2[:], in1=St[:], op=ALU.mult),
                 r=[bG2[j][gs], bS], w=[B("tf")])
            k.op("gpsimd", lambda e: e.tensor_tensor(out=hb[ti][:], in0=T("te")[:], in1=T("tf")[:], op=ALU.subtract),
                 r=[B("te"), B("tf")], w=[bhb[ti]])
            k.op("tensor", lambda e: e.matmul(psy[ys][:], lhsT=L3[j][0][:], rhs=hb[ti][:], start=(j == 0), stop=(j == 3)),
                 r=[L3[j][1], bhb[ti]], w=[bpsy[ys]])
        Y = lambda nm: yt[nm][ys]
        BY = lambda nm: byt[nm][ys]
        V(lambda e: e.scalar_tensor_tensor(out=Y("y1")[:], in0=uf[us][:], scalar=P_d[:, 0:1], in1=psy[ys][:],
                                           op0=ALU.mult, op1=ALU.add), [buf_uf[us], b_d, bpsy[ys]], [BY("y1")])
        gelu_tanh(k, Y("y1"), BY("y1"), Y("y2"), BY("y2"), Y("y3"), BY("y3"), Y("yo"), BY("yo"))
        k.dma("sync", out=out[:, tt * 512:(tt + 1) * 512], in_=Y("yo")[:], r=[BY("yo")])
    return k.finish()


def gelu_tanh(k, x, bx, t1, bt1, t2, bt2, o, bo, eng="gpsimd"):
    k.op(eng, lambda e: e.tensor_tensor(out=t1[:], in0=x[:], in1=x[:], op=ALU.mult), r=[bx], w=[bt1])
    k.op(eng, lambda e: e.tensor_scalar(out=t1[:], in0=t1[:], scalar1=0.044715, scalar2=1.0, op0=ALU.mult, op1=ALU.add),
         r=[bt1], w=[bt1])
    k.op(eng, lambda e: e.tensor_tensor(out=t2[:], in0=t1[:], in1=x[:], op=ALU.mult), r=[bt1, bx], w=[bt2])
    k.op("scalar", lambda e: e.activation(out=t1[:], in_=t2[:], func=AF.Sigmoid, scale=1.5957691216057308),
         r=[bt2], w=[bt1])
    k.op(eng, lambda e: e.tensor_tensor(out=o[:], in0=x[:], in1=t1[:], op=ALU.mult), r=[bx, bt1], w=[bo])


def s5_inputs(inp, layer, projT, c):
    g = slice(4 * c, 4 * c + 4)
    a_re = inp["s5_a_re"][layer][g]
    a_im = inp["s5_a_im"][layer][g]
    are2 = np.ascontiguousarray(np.concatenate([a_re, a_re], axis=1).T)
    aim2 = np.ascontiguousarray(np.concatenate([a_im, a_im], axis=1).T)
    ldt = np.ascontiguousarray(np.broadcast_to(inp["s5_log_dt"][layer][g][None, :], (128, 4)))
    b_re = inp["s5_b_re"][layer][g]
    b_im = inp["s5_b_im"][layer][g]
    BRI = np.concatenate([b_re, b_im], axis=1)
    BIR = np.concatenate([b_im, b_re], axis=1)
    c_re = inp["s5_c_re"][layer][g]
    c_im = inp["s5_c_im"][layer][g]
    CRI = np.concatenate([c_re.transpose(0, 2, 1), c_im.transpose(0, 2, 1)], axis=1)

    def lay(a):
        return np.ascontiguousarray(a.transpose(1, 0, 2).reshape(128, 64))

    sgn = np.concatenate([-np.ones((64, 1), np.float32), np.ones((64, 1), np.float32)], axis=0)
    return {
        "u": np.ascontiguousarray(projT[64 * c:64 * c + 64, :]),
        "are2": are2, "aim2": aim2, "ldt": ldt,
        "BRI": lay(BRI), "BIR": lay(BIR), "CRI": lay(CRI),
        "dvec": np.ascontiguousarray(inp["s5_d"][layer][64 * c:64 * c + 64].reshape(64, 1)),
        "sgn": sgn, "ident": np.eye(128, dtype=np.float32),
    }


MLA_SCALE = 192 ** -0.5


def build_MLA():
    k = KB()
    NT = SEQ // 512
    cq = k.din("cq", [512, SEQ])
    ckv = k.din("ckv", [256, SEQ])
    kr = k.din("kr", [64, SEQ])
    krsw = k.din("krsw", [64, SEQ])
    cosT = k.din("cosT", [64, SEQ])
    sinT = k.din("sinT", [64, SEQ])
    d_qn = k.din("qn", [128, 4])
    d_kvn = k.din("kvn", [128, 2])
    d_wq = k.din("wq", [512, 192])
    d_wqsw = k.din("wqsw", [512, 64])
    d_wkv = k.din("wkv", [256, 256])
    d_sgn = k.din("sgn64", [64, 1])
    d_mask = k.din("mask", [128, 4, 512])
    out = k.dout("y", [SEQ, 128])

    def V(fn, r, w):
        return k.op("vector", fn, r=r, w=w)

    def G(fn, r, w):
        return k.op("gpsimd", fn, r=r, w=w)

    def A(fn, r, w):
        return k.op("scalar", fn, r=r, w=w)

    def PE(fn, r, w):
        return k.op("tensor", fn, r=r, w=w)

    qn = k.sb("qn", [128, 4], F32); bqn = Buf()
    k.dma("sync", out=qn[:], in_=d_qn, w=[bqn])
    kvn = k.sb("kvn", [128, 2], F32); bkvn = Buf()
    k.dma("sync", out=kvn[:], in_=d_kvn, w=[bkvn])
    sgn = k.sb("sgn", [64, 1], F32); bsgn = Buf()
    k.dma("sync", out=sgn[:], in_=d_sgn, w=[bsgn])
    wq = k.sb("wq", [128, 4, 192], BF16); bwq = Buf()
    k.dma("gpsimd", out=wq[:], in_=d_wq.rearrange("(kc p) m -> p kc m", p=128), w=[bwq])
    wqsw = k.sb("wqsw", [128, 4, 64], BF16); bwqsw = Buf()
    k.dma("gpsimd", out=wqsw[:], in_=d_wqsw.rearrange("(kc p) m -> p kc m", p=128), w=[bwqsw])
    G(lambda e: e.tensor_scalar(out=wqsw[:, :, 0:32], in0=wqsw[:, :, 0:32], scalar1=-1.0, scalar2=None, op0=ALU.mult),
      [bwqsw], [bwqsw])
    wkv = k.sb("wkv", [128, 2, 256], BF16); bwkv = Buf()
    k.dma("gpsimd", out=wkv[:], in_=d_wkv.rearrange("(kc p) m -> p kc m", p=128), w=[bwkv])
    maskf = k.sb("maskf", [128, 4, 512], F32); bmaskf = Buf()
    k.dma("sync", out=maskf[:], in_=d_mask, w=[bmaskf])
    mask = k.sb("mask", [128, 4, 512], BF16); bmask = Buf()
    G(lambda e: e.tensor_copy(out=mask[:], in_=maskf[:]), [bmaskf], [bmask])
    ones_f = k.sb("ones_f", [128, 128], F32); bof = Buf()
    V(lambda e: e.memset(ones_f[:], 1.0), [], [bof])
    ones_b = k.sb("ones_b", [128, 128], BF16); bob = Buf()
    V(lambda e: e.memset(ones_b[:], 1.0), [], [bob])
    epsq = k.sb("epsq", [128, 1], F32); beps = Buf()
    V(lambda e: e.memset(epsq[:], RMS_EPS), [], [beps])
    kmax2 = k.sb("kmax2", [128, 1], F32); bkm = Buf()
    V(lambda e: e.memset(kmax2[:], 0.0), [], [bkm])

    KTn = k.sb("KTn", [128, SEQ], BF16)
    KTr = k.sb("KTr", [65, SEQ], BF16)
    Vt = k.sb("Vt", [128, SEQ // 128, 129], BF16)
    bKT = bufs(NT, "kt")
    bVt = bufs(NT, "vt")
    bKTones = Buf()
    V(lambda e: e.memset(KTr[64:65, :], 1.0), [], [bKTones])
    bVones = Buf()
    G(lambda e: e.memset(Vt[:, :, 128:129], 1.0), [], [bVones])

    NPS = 4
    psg = [k.ps("psg", [128, 512]) for _ in range(NPS)]
    bpsg = bufs(NPS, "psg")
    pso = [k.ps("pso", [128, 512]) for _ in range(4)]
    bpso = bufs(4, "pso")
    psc = [0]

    def nps():
        i = psc[0] % NPS
        psc[0] += 1
        return psg[i], bpsg[i]

    xin = [k.sb("xin", [128, 4, 512], F32) for _ in range(2)]; bxin = bufs(2)
    sq = [k.sb("sq", [128, 4, 512], F32) for _ in range(2)]; bsq = bufs(2)
    rr = [k.sb("rr", [128, 512], F32) for _ in range(2)]; brr = bufs(2)
    xn = [k.sb("xn", [128, 4, 512], BF16) for _ in range(2)]; bxn = bufs(2)
    rin = [k.sb("rin", [64, 2, 512], F32) for _ in range(2)]; brin = bufs(2)
    cs = [k.sb("cs", [64, 2, 512], F32) for _ in range(2)]; bcs = bufs(2)
    t1 = [k.sb("t1", [64, 512], F32) for _ in range(2)]; bt1 = bufs(2)
    t2 = [k.sb("t2", [64, 512], F32) for _ in range(2)]; bt2 = bufs(2)
    s2n = [k.sb("s2n", [128, 512], BF16) for _ in range(2)]; bs2n = bufs(2)
    s2r = [k.sb("s2r", [64, 512], BF16) for _ in range(2)]; bs2r = bufs(2)
    mx = [k.sb("mx", [128, 1], F32) for _ in range(2)]; bmx = bufs(2)
    QTn = [k.sb("QTn", [128, 512], BF16) for _ in range(2)]; bQTn = bufs(2)
    QTr = [k.sb("QTr", [65, 512], BF16) for _ in range(2)]; bQTr = bufs(2)

    def rms_norm_tile(src, nkc, normw, bnormw, i, width):
        A(lambda e: e.activation(out=sq[i][:, 0:nkc, :], in_=xin[i][:, 0:nkc, :], func=AF.Square), [bxin[i]], [bsq[i]])
        ps, bps = nps()
        for kc in range(nkc):
            PE(lambda e: e.matmul(ps[:], lhsT=ones_f[:], rhs=sq[i][:, kc, :], start=(kc == 0), stop=(kc == nkc - 1)),
               [bof, bsq[i]], [bps])
        A(lambda e: e.activation(out=rr[i][:], in_=ps[:], func=AF.Sqrt, scale=1.0 / width, bias=epsq[:, 0:1]),
          [bps, beps], [brr[i]])
        V(lambda e: e.reciprocal(out=rr[i][:], in_=rr[i][:]), [brr[i]], [brr[i]])
        for kc in range(nkc):
            V(lambda e: e.scalar_tensor_tensor(out=xn[i][:, kc, :], in0=xin[i][:, kc, :], scalar=normw[:, kc:kc + 1],
                                               in1=rr[i][:], op0=ALU.mult, op1=ALU.mult),
              [bxin[i], bnormw, brr[i]], [bxn[i]])

    for tt in range(NT):
        i = tt % 2
        tsl = slice(tt * 512, (tt + 1) * 512)
        k.dma("sync", out=xin[i][:, 0:2, :], in_=ckv.rearrange("(kc p) t -> p kc t", p=128)[:, :, tsl], w=[bxin[i]])
        k.dma("sync", out=rin[i][:, 0, :], in_=kr[:, tsl], w=[brin[i]])
        k.dma("sync", out=rin[i][:, 1, :], in_=krsw[:, tsl], w=[brin[i]])
        k.dma("sync", out=cs[i][:, 0, :], in_=cosT[:, tsl], w=[bcs[i]])
        k.dma("sync", out=cs[i][:, 1, :], in_=sinT[:, tsl], w=[bcs[i]])
        rms_norm_tile(ckv, 2, kvn, bkvn, i, 256.0)
        ps, bps = nps()
        for kc in range(2):
            PE(lambda e: e.matmul(ps[:], lhsT=wkv[:, kc, 0:128], rhs=xn[i][:, kc, :], start=(kc == 0), stop=(kc == 1)),
               [bwkv, bxn[i]], [bps])
        A(lambda e: e.copy(out=KTn[:, tsl], in_=ps[:]), [bps], [bKT[tt]])
        G(lambda e: e.tensor_tensor(out=t1[i][:], in0=rin[i][:, 0, :], in1=cs[i][:, 0, :], op=ALU.mult),
          [brin[i], bcs[i]], [bt1[i]])
        V(lambda e: e.scalar_tensor_tensor(out=t2[i][:], in0=rin[i][:, 1, :], scalar=sgn[:, 0:1], in1=cs[i][:, 1, :],
                                           op0=ALU.mult, op1=ALU.mult), [brin[i], bsgn, bcs[i]], [bt2[i]])
        G(lambda e: e.tensor_tensor(out=KTr[0:64, tsl], in0=t1[i][:], in1=t2[i][:], op=ALU.add),
          [bt1[i], bt2[i]], [bKT[tt]])
        G(lambda e: e.tensor_tensor(out=s2n[i][:], in0=KTn[:, tsl], in1=KTn[:, tsl], op=ALU.mult), [bKT[tt]], [bs2n[i]])
        G(lambda e: e.tensor_tensor(out=s2r[i][:], in0=KTr[0:64, tsl], in1=KTr[0:64, tsl], op=ALU.mult), [bKT[tt]], [bs2r[i]])
        ps2, bps2 = nps()
        PE(lambda e: e.matmul(ps2[:], lhsT=ones_b[:], rhs=s2n[i][:], start=True, stop=False), [bob, bs2n[i]], [bps2])
        PE(lambda e: e.matmul(ps2[:], lhsT=ones_b[0:64, :], rhs=s2r[i][:], start=False, stop=True), [bob, bs2r[i]], [bps2])
        V(lambda e: e.reduce_max(out=mx[i][:], in_=ps2[:], axis=AX.X), [bps2], [bmx[i]])
        V(lambda e: e.tensor_tensor(out=kmax2[:], in0=kmax2[:], in1=mx[i][:], op=ALU.max), [bkm, bmx[i]], [bkm])
        ps3, bps3 = nps()
        for b4 in range(4):
            for kc in range(2):
                PE(lambda e: e.matmul(ps3[:, b4 * 128:(b4 + 1) * 128], lhsT=xn[i][:, kc, b4 * 128:(b4 + 1) * 128],
                                      rhs=wkv[:, kc, 128:256], start=(kc == 0), stop=(kc == 1)),
                   [bwkv, bxn[i]], [bps3])
        V(lambda e: e.tensor_copy(out=Vt[:, tt * 4:(tt + 1) * 4, 0:128], in_=ps3[:].rearrange("p (b d) -> p b d", b=4)),
          [bps3], [bVt[tt]])

    NPT = 3
    PT = [k.sb("PT", [128, 512], BF16) for _ in range(NPT)]; bPT = bufs(NPT)
    rec = [k.sb("rec", [128, 1], F32) for _ in range(4)]; brec = bufs(4)
    ot = [k.sb("ot", [128, 128], F32) for _ in range(4)]; bot = bufs(4)
    ptc = 0
    for qt in range(NT):
        i = qt % 2
        tsl = slice(qt * 512, (qt + 1) * 512)
        k.dma("sync", out=xin[i][:], in_=cq.rearrange("(kc p) t -> p kc t", p=128)[:, :, tsl], w=[bxin[i]])
        k.dma("sync", out=cs[i][:, 0, :], in_=cosT[:, tsl], w=[bcs[i]])
        k.dma("sync", out=cs[i][:, 1, :], in_=sinT[:, tsl], w=[bcs[i]])
        rms_norm_tile(cq, 4, qn, bqn, i, 512.0)
        ps, bps = nps()
        for kc in range(4):
            PE(lambda e: e.matmul(ps[:], lhsT=wq[:, kc, 0:128], rhs=xn[i][:, kc, :], start=(kc == 0), stop=(kc == 3)),
               [bwq, bxn[i]], [bps])
        A(lambda e: e.activation(out=QTn[i][:], in_=ps[:], func=AF.Identity, scale=MLA_SCALE), [bps], [bQTn[i]])
        psr, bpsr = nps()
        for kc in range(4):
            PE(lambda e: e.matmul(psr[0:64, :], lhsT=wq[:, kc, 128:192], rhs=xn[i][:, kc, :], start=(kc == 0), stop=(kc == 3)),
               [bwq, bxn[i]], [bpsr])
        pss, bpss = nps()
        for kc in range(4):
            PE(lambda e: e.matmul(pss[0:64, :], lhsT=wqsw[:, kc, :], rhs=xn[i][:, kc, :], start=(kc == 0), stop=(kc == 3)),
               [bwqsw, bxn[i]], [bpss])
        V(lambda e: e.tensor_tensor(out=t1[i][:], in0=psr[0:64, :], in1=cs[i][:, 0, :], op=ALU.mult), [bpsr, bcs[i]], [bt1[i]])
        V(lambda e: e.tensor_tensor(out=t2[i][:], in0=pss[0:64, :], in1=cs[i][:, 1, :], op=ALU.mult), [bpss, bcs[i]], [bt2[i]])
        G(lambda e: e.tensor_tensor(out=t1[i][:], in0=t1[i][:], in1=t2[i][:], op=ALU.add), [bt1[i], bt2[i]], [bt1[i]])
        A(lambda e: e.activation(out=QTr[i][0:64, :], in_=t1[i][:], func=AF.Identity, scale=MLA_SCALE), [bt1[i]], [bQTr[i]])
        G(lambda e: e.tensor_tensor(out=s2n[i][:], in0=QTn[i][:], in1=QTn[i][:], op=ALU.mult), [bQTn[i]], [bs2n[i]])
        G(lambda e: e.tensor_tensor(out=s2r[i][:], in0=QTr[i][0:64, :], in1=QTr[i][0:64, :], op=ALU.mult), [bQTr[i]], [bs2r[i]])
        ps2, bps2 = nps()
        PE(lambda e: e.matmul(ps2[:], lhsT=ones_b[:], rhs=s2n[i][:], start=True, stop=False), [bob, bs2n[i]], [bps2])
        PE(lambda e: e.matmul(ps2[:], lhsT=ones_b[0:64, :], rhs=s2r[i][:], start=False, stop=True), [bob, bs2r[i]], [bps2])
        A(lambda e: e.activation(out=rr[i][64:65, :], in_=ps2[64:65, :], func=AF.Sqrt, scale=kmax2[64:65, 0:1]),
          [bps2, bkm], [brr[i]])
        V(lambda e: e.tensor_scalar(out=QTr[i][64:65, :], in0=rr[i][64:65, :], scalar1=-1.0, scalar2=None, op0=ALU.mult),
          [brr[i]], [bQTr[i]])
        nkb = 4 * qt + 4

        def issue_S(kb):
            ps_, bps_ = nps()
            ksl = slice(kb * 128, (kb + 1) * 128)
            PE(lambda e: e.matmul(ps_[:], lhsT=KTn[:, ksl], rhs=QTn[i][:], start=True, stop=False),
               [bKT[kb // 4], bQTn[i]], [bps_])
            PE(lambda e: e.matmul(ps_[:], lhsT=KTr[0:65, ksl], rhs=QTr[i][0:65, :], start=False, stop=True),
               [bKT[kb // 4], bKTones, bQTr[i]], [bps_])
            return ps_, bps_

        cur = issue_S(0)
        for kb in range(nkb):
            nxt = issue_S(kb + 1) if kb + 1 < nkb else None
            ps_, bps_ = cur
            pi = ptc % NPT
            ptc += 1
            A(lambda e: e.activation(out=PT[pi][:], in_=ps_[:], func=AF.Exp), [bps_], [bPT[pi]])
            di = kb - 4 * qt
            if di >= 0:
                G(lambda e: e.tensor_tensor(out=PT[pi][:], in0=PT[pi][:], in1=mask[:, di, :], op=ALU.mult),
                  [bPT[pi], bmask], [bPT[pi]])
            for jq in range(4):
                if di > jq:
                    continue
                last = (kb == 4 * qt + jq)
                PE(lambda e: e.matmul(pso[jq][:, 0:129], lhsT=PT[pi][:, jq * 128:(jq + 1) * 128], rhs=Vt[:, kb, :],
                                      start=(kb == 0), stop=last),
                   [bPT[pi], bVt[kb // 4], bVones], [bpso[jq]])
            cur = nxt
        for jq in range(4):
            V(lambda e: e.reciprocal(out=rec[jq][:], in_=pso[jq][:, 128:129]), [bpso[jq]], [brec[jq]])
            V(lambda e: e.tensor_scalar(out=ot[jq][:], in0=pso[jq][:, 0:128], scalar1=rec[jq][:, 0:1], scalar2=None,
                                        op0=ALU.mult), [bpso[jq], brec[jq]], [bot[jq]])
            r0 = qt * 512 + jq * 128
            k.dma("sync", out=out[r0:r0 + 128, :], in_=ot[jq][:], r=[bot[jq]])
    return k.finish()


def rope_tables():
    pos = np.arange(SEQ, dtype=np.float32)
    inv_freq = (np.float32(10000.0) ** (-np.arange(0, 64, 2, dtype=np.float32) / np.float32(64))).astype(np.float32)
    ang = (pos[:, None] * inv_freq[None, :]).astype(np.float32)
    cos = np.cos(ang).astype(np.float32)
    sin = np.sin(ang).astype(np.float32)
    cosT = np.ascontiguousarray(np.concatenate([cos, cos], axis=1).T)
    sinT = np.ascontiguousarray(np.concatenate([sin, sin], axis=1).T)
    return cosT, sinT


def causal_masks():
    kk = np.arange(128)[:, None, None]
    ii = np.arange(4)[None, :, None]
    jj = np.arange(512)[None, None, :]
    return (128 * ii + kk <= jj).astype(np.float32)


def mla_inputs(inp, layer, projT, c, consts):
    cosT, sinT, mask = consts
    kr = projT[1280:1344]
    wq = inp["mla_w_uq"][layer][:, c, :]
    return {
        "cq": np.ascontiguousarray(projT[512:1024]),
        "ckv": np.ascontiguousarray(projT[1024:1280]),
        "kr": np.ascontiguousarray(kr),
        "krsw": np.ascontiguousarray(np.concatenate([kr[32:64], kr[0:32]], axis=0)),
        "cosT": cosT, "sinT": sinT,
        "qn": np.ascontiguousarray(inp["mla_q_norm"][layer].reshape(4, 128).T),
        "kvn": np.ascontiguousarray(inp["mla_kv_norm"][layer].reshape(2, 128).T),
        "wq": np.ascontiguousarray(wq),
        "wqsw": np.ascontiguousarray(np.concatenate([wq[:, 160:192], wq[:, 128:160]], axis=1)),
        "wkv": np.ascontiguousarray(inp["mla_w_ukv"][layer][:, c, :]),
        "sgn64": np.concatenate([-np.ones((32, 1), np.float32), np.ones((32, 1), np.float32)], axis=0),
        "mask": mask,
    }


def build_HG(SEQ=SEQ, dbg=9):
    k = KB()
    NT = SEQ // 512
    hq = k.din("hq", [128, SEQ])
    hf = k.din("hf", [128, SEQ])
    hi = k.din("hi", [64, SEQ])
    d_lbl = k.din("lbl", [128, 2])
    d_sel = k.din("sel", [128, 1])
    d_id = k.din("ident", [128, 128])
    d_bd = k.din("bdmask", [128, 128])
    d_rm = k.din("rmask", [128, 512])
    out = k.dout("ohg", [64, SEQ])

    def V(fn, r, w):
        return k.op("vector", fn, r=r, w=w)

    def G(fn, r, w):
        return k.op("gpsimd", fn, r=r, w=w)

    def A(fn, r, w):
        return k.op("scalar", fn, r=r, w=w)

    def PE(fn, r, w):
        return k.op("tensor", fn, r=r, w=w)

    def load(src, shape):
        t = k.sb("p", shape, F32)
        b = Buf()
        k.dma("sync", out=t[:], in_=src, w=[b])
        return t, b

    lbl, blbl = load(d_lbl, [128, 2])
    sel, bsel = load(d_sel, [128, 1])
    idf, bidf = load(d_id, [128, 128])
    bdf, bbdf = load(d_bd, [128, 128])
    rmask, brm = load(d_rm, [128, 512])
    idb = k.sb("idb", [128, 128], BF16); bidb = Buf()
    V(lambda e: e.tensor_copy(out=idb[:], in_=idf[:]), [bidf], [bidb])
    pc = k.sb("pc", [128, 8], F32); bpc = Buf()
    V(lambda e: e.tensor_tensor(out=pc[:, 0:1], in0=lbl[:, 1:2], in1=lbl[:, 0:1], op=ALU.subtract), [blbl], [bpc])
    A(lambda e: e.activation(out=pc[:, 1:2], in_=pc[:, 0:1], func=AF.Sigmoid), [bpc], [bpc])
    V(lambda e: e.tensor_tensor(out=pc[:, 2:3], in0=pc[:, 1:2], in1=sel[:, 0:1], op=ALU.mult), [bpc, bsel], [bpc])
    V(lambda e: e.tensor_scalar(out=pc[:, 3:4], in0=pc[:, 2:3], scalar1=-1.0, scalar2=1.0, op0=ALU.mult, op1=ALU.add),
      [bpc], [bpc])
    V(lambda e: e.tensor_scalar(out=pc[:, 4:5], in0=pc[:, 3:4], scalar1=-1.0, scalar2=None, op0=ALU.mult), [bpc], [bpc])
    LB, OML, NOML = pc[:, 2:3], pc[:, 3:4], pc[:, 4:5]

    state = k.sb("state", [128, 64], F32); bst = Buf()
    V(lambda e: e.memset(state[:], 0.0), [], [bst])

    def dbl(name, shape, dt):
        return [k.sb(name, shape, dt) for _ in range(2)], bufs(2)

    xq, bxq = dbl("xq", [128, 512], F32)
    xf, bxf = dbl("xf", [128, 512], F32)
    xi, bxi = dbl("xi", [64, 512], F32)
    q, bq = dbl("q", [128, 512], F32)
    sg, bsg = dbl("sg", [128, 512], F32)
    ff, bff = dbl("ff", [128, 512], F32)
    kk, bkk = dbl("kk", [128, 512], F32)
    bb, bbb = dbl("bb", [128, 512], F32)
    d1, bd1 = dbl("d1", [128, 512], F32)
    eq, beq = dbl("eq", [128, 512], F32)
    ek, bek = dbl("ek", [128, 512], F32)
    qt_, bqt = dbl("qt", [128, 512], BF16)
    kt_, bkt = dbl("kt", [128, 512], BF16)
    vb, bvb = dbl("vb", [64, 512], BF16)
    sm, bsm = dbl("sm", [128, 4, 8], F32)
    ktok, bktok = dbl("ktok", [64, 8, 128], BF16)
    vtok, bvtok = dbl("vtok", [64, 8, 64], BF16)
    Sb, bSb = dbl("Sb", [128, 8, 64], BF16)
    tmpk, btmpk = dbl("tmpk", [128, 64], F32)
    at, bat = dbl("at", [64, 64], BF16)
    ob, bob_ = dbl("ob", [64, 512], F32)
    ps_kt_full = [k.ps("ps_kt", [128, 1024], BF16) for _ in range(1)]; bps_kt = bufs(1)
    ps_kt = [t[0:64, :] for t in ps_kt_full]
    ps_vt_full = [k.ps("ps_vt", [128, 1024], BF16) for _ in range(1)]; bps_vt = bufs(1)
    ps_vt = [t[0:64, 0:512] for t in ps_vt_full]
    ps_kv = [k.ps("ps_kv", [128, 512]) for _ in range(1)]; bps_kv = bufs(1)
    ps_at_full = [k.ps("ps_at", [128, 512]) for _ in range(2)]; bps_at = bufs(2)
    ps_at = [t[0:64, 0:64] for t in ps_at_full]
    ps_o_full = [k.ps("ps_o", [128, 512]) for _ in range(2)]; bps_o = bufs(2)
    ps_o = [t[0:64, :] for t in ps_o_full]
    atc = 0
    for tt in range(NT):
        i = tt % 2
        tsl = slice(tt * 512, (tt + 1) * 512)
        k.dma("sync", out=xq[i][:], in_=hq[:, tsl], w=[bxq[i]])
        k.dma("sync", out=xf[i][:], in_=hf[:, tsl], w=[bxf[i]])
        k.dma("sync", out=xi[i][:], in_=hi[:, tsl], w=[bxi[i]])
        A(lambda e: e.activation(out=q[i][:], in_=xq[i][:], func=AF.Silu), [bxq[i]], [bq[i]])
        A(lambda e: e.activation(out=sg[i][:], in_=xf[i][:], func=AF.Sigmoid), [bxf[i]], [bsg[i]])
        V(lambda e: e.tensor_scalar(out=ff[i][:], in0=sg[i][:], scalar1=OML, scalar2=LB, op0=ALU.mult, op1=ALU.add),
          [bsg[i], bpc], [bff[i]])
        G(lambda e: e.tensor_scalar(out=kk[i][:], in0=sg[i][:], scalar1=NOML, scalar2=OML, op0=ALU.mult, op1=ALU.add),
          [bsg[i], bpc], [bkk[i]])
        A(lambda e: e.activation(out=ff[i][:], in_=ff[i][:], func=AF.Ln), [bff[i]], [bff[i]])
        V(lambda e: e.tensor_tensor_scan(out=bb[i][:], data0=rmask[:], data1=ff[i][:], initial=0.0, op0=ALU.mult, op1=ALU.add),
          [brm, bff[i]], [bbb[i]])
        b3 = bb[i][:].rearrange("p (n c) -> p n c", c=64)
        for n in range(8):
            eng = V if n % 2 == 0 else G
            eng(lambda e: e.tensor_scalar(out=d1[i][:, n * 64:(n + 1) * 64], in0=bb[i][:, n * 64:(n + 1) * 64],
                                          scalar1=bb[i][:, n * 64 + 31:n * 64 + 32], scalar2=None, op0=ALU.subtract),
                [bbb[i]], [bd1[i]])
        A(lambda e: e.activation(out=eq[i][:], in_=d1[i][:], func=AF.Exp), [bd1[i]], [beq[i]])
        A(lambda e: e.activation(out=ek[i][:], in_=d1[i][:], func=AF.Exp, scale=-1.0), [bd1[i]], [bek[i]])
        V(lambda e: e.tensor_tensor(out=qt_[i][:], in0=q[i][:], in1=eq[i][:], op=ALU.mult), [bq[i], beq[i]], [bqt[i]])
        G(lambda e: e.tensor_tensor(out=kt_[i][:], in0=kk[i][:], in1=ek[i][:], op=ALU.mult), [bkk[i], bek[i]], [bkt[i]])
        G(lambda e: e.tensor_copy(out=vb[i][:], in_=xi[i][:]), [bxi[i]], [bvb[i]])
        d13 = d1[i][:].rearrange("p (n c) -> p n c", c=64)
        V(lambda e: e.tensor_copy(out=sm[i][:, 0, :], in_=d13[:, :, 63]), [bd1[i]], [bsm[i]])
        A(lambda e: e.activation(out=sm[i][:, 1, :], in_=b3[:, :, 63], func=AF.Exp), [bbb[i], bsm[i]], [bsm[i]])
        A(lambda e: e.activation(out=sm[i][:, 2, :], in_=sm[i][:, 0, :], func=AF.Exp), [bsm[i]], [bsm[i]])
        A(lambda e: e.activation(out=sm[i][:, 3, :], in_=b3[:, :, 31], func=AF.Exp), [bbb[i], bsm[i]], [bsm[i]])
        if dbg <= 1:
            G(lambda e: e.tensor_copy(out=ob[i][:], in_=q[i][0:64, :]), [bq[i], bqt[i], bkt[i], bsm[i], bvb[i]], [bob_[i]])
            k.dma('sync', out=out[:, tsl], in_=ob[i][:], r=[bob_[i]])
            continue
        for n in range(8):
            PE(lambda e: e.transpose(ps_kt[0][:, n * 128:(n + 1) * 128], kt_[i][:, n * 64:(n + 1) * 64], idb[:]),
               [bkt[i], bidb], [bps_kt[0]])
        V(lambda e: e.tensor_copy(out=ktok[i][:], in_=ps_kt[0].rearrange("p (b d) -> p b d", b=8)), [bps_kt[0]], [bktok[i]])
        for n in range(8):
            PE(lambda e: e.transpose(ps_vt[0][:, n * 64:(n + 1) * 64], vb[i][:, n * 64:(n + 1) * 64], idb[0:64, 0:64]),
               [bvb[i], bidb], [bps_vt[0]])
        V(lambda e: e.tensor_copy(out=vtok[i][:], in_=ps_vt[0].rearrange("p (b d) -> p b d", b=8)), [bps_vt[0]], [bvtok[i]])
        for n in range(8):
            PE(lambda e: e.matmul(ps_kv[0][:, n * 64:(n + 1) * 64], lhsT=ktok[i][:, n, :], rhs=vtok[i][:, n, :],
                                  start=True, stop=True), [bktok[i], bvtok[i]], [bps_kv[0]])
        for n in range(8):
            V(lambda e: e.tensor_scalar(out=Sb[i][:, n, :], in0=state[:], scalar1=sm[i][:, 3, n:n + 1], scalar2=None,
                                        op0=ALU.mult), [bst, bsm[i]], [bSb[i]])
            V(lambda e: e.tensor_scalar(out=tmpk[n % 2][:], in0=ps_kv[0][:, n * 64:(n + 1) * 64], scalar1=sm[i][:, 2, n:n + 1],
                                        scalar2=None, op0=ALU.mult), [bps_kv[0], bsm[i]], [btmpk[n % 2]])
            V(lambda e: e.scalar_tensor_tensor(out=state[:], in0=state[:], scalar=sm[i][:, 1, n:n + 1], in1=tmpk[n % 2][:],
                                               op0=ALU.mult, op1=ALU.add), [bst, bsm[i], btmpk[n % 2]], [bst])
        oi = tt % 2
        for n in range(8):
            ai = atc % 2
            atc += 1
            cs_ = slice(n * 64, (n + 1) * 64)
            PE(lambda e: e.matmul(ps_at[ai], lhsT=kt_[i][:, cs_], rhs=qt_[i][:, cs_], start=True, stop=True),
               [bkt[i], bqt[i]], [bps_at[ai]])
            V(lambda e: e.tensor_tensor(out=at[ai][:], in0=ps_at[ai], in1=bdf[0:64, 0:64], op=ALU.mult), [bps_at[ai], bbdf], [bat[ai]])
            PE(lambda e: e.matmul(ps_o[oi][:, cs_], lhsT=vtok[i][:, n, :], rhs=at[ai][:], start=True, stop=False),
               [bvtok[i], bat[ai]], [bps_o[oi]])
            PE(lambda e: e.matmul(ps_o[oi][:, cs_], lhsT=Sb[i][:, n, :], rhs=qt_[i][:, cs_], start=False, stop=True),
               [bSb[i], bqt[i]], [bps_o[oi]])
        A(lambda e: e.copy(out=ob[oi][:], in_=ps_o[oi]), [bps_o[oi]], [bob_[oi]])
        k.dma("sync", out=out[:, tsl], in_=ob[oi][:], r=[bob_[oi]])
    return k.finish()


def hg_consts():
    s = np.arange(128)[:, None]
    t = np.arange(128)[None, :]
    bd = ((s // 64 == t // 64) & (s <= t)).astype(np.float32)
    rm = np.ones((128, 512), np.float32)
    rm[:, ::64] = 0.0
    return bd, rm


def hg_inputs(inp, layer, projT, c, consts):
    bd, rm = consts
    h, half = c // 2, c % 2
    o = 1344
    return {
        "hq": np.ascontiguousarray(projT[o + 128 * h:o + 128 * h + 128]),
        "hf": np.ascontiguousarray(projT[o + 512 + 128 * h:o + 512 + 128 * h + 128]),
        "hi": np.ascontiguousarray(projT[o + 1024 + 128 * h + 64 * half:o + 1024 + 128 * h + 64 * half + 64]),
        "lbl": np.ascontiguousarray(inp["hg_lb_logits"][:, 128 * h:128 * h + 128].T),
        "sel": np.full((128, 1), float(layer), np.float32),
        "ident": np.eye(128, dtype=np.float32),
        "bdmask": bd, "rmask": rm,
    }


class LNCtx:
    def __init__(self, k, ones_f, bof, gam, bgam, bet, bbet):
        self.k = k
        self.ones_f, self.bof = ones_f, bof
        self.gam, self.bgam, self.bet, self.bbet = gam, bgam, bet, bbet
        self.zsq = [k.sb("zsq", [128, 512], F32) for _ in range(2)]
        self.bzsq = bufs(2)
        self.ps1 = k.ps("lnps1", [128, 512]); self.bps1 = Buf()
        self.ps2 = k.ps("lnps2", [128, 512]); self.bps2 = Buf()
        self.mean = k.sb("mean", [128, 512], F32); self.bmean = Buf()
        self.rstd = k.sb("rstd", [128, 512], F32); self.brstd = Buf()
        self.msq = k.sb("msq", [128, 512], F32); self.bmsq = Buf()
        self.eps = k.sb("lneps", [128, 1], F32); self.beps = Buf()
        k.op("vector", lambda e: e.memset(self.eps[:], LN_EPS), [], [self.beps])
        self.t1 = [k.sb("lnt1", [128, 512], F32) for _ in range(2)]; self.bt1 = bufs(2)
        self.t2 = [k.sb("lnt2", [128, 512], F32) for _ in range(2)]; self.bt2 = bufs(2)

    def run(self, z, bz, emit):
        k = self.k
        NC = D // 128
        for mc in range(NC):
            j = mc % 2
            k.op("scalar", lambda e: e.activation(out=self.zsq[j][:], in_=z[:, mc, :], func=AF.Square), [bz[mc]], [self.bzsq[j]])
            k.op("tensor", lambda e: e.matmul(self.ps1[:], lhsT=self.ones_f[:], rhs=z[:, mc, :], start=(mc == 0), stop=(mc == NC - 1)),
                 [self.bof, bz[mc]], [self.bps1])
            k.op("tensor", lambda e: e.matmul(self.ps2[:], lhsT=self.ones_f[:], rhs=self.zsq[j][:], start=(mc == 0), stop=(mc == NC - 1)),
                 [self.bof, self.bzsq[j]], [self.bps2])
        k.op("scalar", lambda e: e.activation(out=self.mean[:], in_=self.ps1[:], func=AF.Identity, scale=1.0 / D), [self.bps1], [self.bmean])
        k.op("vector", lambda e: e.tensor_tensor(out=self.msq[:], in0=self.mean[:], in1=self.mean[:], op=ALU.mult), [self.bmean], [self.bmsq])
        k.op("vector", lambda e: e.scalar_tensor_tensor(out=self.rstd[:], in0=self.ps2[:], scalar=1.0 / D, in1=self.msq[:],
                                                        op0=ALU.mult, op1=ALU.subtract), [self.bps2, self.bmsq], [self.brstd])
        k.op("scalar", lambda e: e.activation(out=self.rstd[:], in_=self.rstd[:], func=AF.Sqrt, bias=self.eps[:, 0:1]),
             [self.brstd, self.beps], [self.brstd])
        k.op("vector", lambda e: e.reciprocal(out=self.rstd[:], in_=self.rstd[:]), [self.brstd], [self.brstd])
        for mc in range(NC):
            j = mc % 2
            k.op("vector", lambda e: e.tensor_tensor(out=self.t1[j][:], in0=z[:, mc, :], in1=self.mean[:], op=ALU.subtract),
                 [bz[mc], self.bmean], [self.bt1[j]])
            k.op("gpsimd", lambda e: e.tensor_tensor(out=self.t1[j][:], in0=self.t1[j][:], in1=self.rstd[:], op=ALU.mult),
                 [self.bt1[j], self.brstd], [self.bt1[j]])
            k.op("scalar", lambda e: e.activation(out=self.t2[j][:], in_=self.t1[j][:], func=AF.Identity,
                                                  scale=self.gam[:, mc:mc + 1], bias=self.bet[:, mc:mc + 1]),
                 [self.bt1[j], self.bgam, self.bbet], [self.bt2[j]])
            emit(mc, self.t2[j], self.bt2[j])


def build_C1(with_router):
    k = KB()
    NT = TOK // 512
    ys5 = k.din("ys5", [512, TOK])
    ymla = k.din("ymla", [1024, TOK])
    ohg = k.din("ohg", [512, TOK])
    hg = k.din("hg", [512, TOK])
    xT = k.din("xT", [D, TOK])
    d_wglu = k.din("wglu", [512, 512])
    d_bglu = k.din("bglu", [128, 4])
    d_onorm = k.din("onorm", [128, 1])
    d_wout = k.din("wout", [D, D])
    d_g = k.din("lng", [128, 16])
    d_b = k.din("lnb", [128, 16])
    x1o = k.dout("x1T", [D, TOK])
    x1bo = k.dout("x1bf", [D, TOK], BF16)
    if with_router:
        d_wr = k.din("wr", [D, NEXP])
        gout = k.dout("gates", [TOK, NEXP])

    def V(fn, r, w):
        return k.op("vector", fn, r=r, w=w)

    def G(fn, r, w):
        return k.op("gpsimd", fn, r=r, w=w)

    def A(fn, r, w):
        return k.op("scalar", fn, r=r, w=w)

    def PE(fn, r, w):
        return k.op("tensor", fn, r=r, w=w)

    def load(src, shape, q="sync", dt=F32):
        t = k.sb("p", shape, dt)
        b = Buf()
        k.dma(q, out=t[:], in_=src, w=[b])
        return t, b

    bglu, bbglu = load(d_bglu, [128, 4])
    onorm, bonorm = load(d_onorm, [128, 1])
    gam, bgam = load(d_g, [128, 16])
    bet, bbet = load(d_b, [128, 16])
    wglu, bwglu = load(d_wglu.rearrange("(kc p) m -> p kc m", p=128), [128, 4, 512], "gpsimd", BF16)
    wout = k.sb("wout", [128, 16, D], BF16)
    bwout = bufs(16)
    for kc in range(16):
        k.dma("gpsimd", out=wout[:, kc, :], in_=d_wout[kc * 128:(kc + 1) * 128, :], w=[bwout[kc]])
    if with_router:
        wr, bwr = load(d_wr.rearrange("(kc p) m -> p kc m", p=128), [128, 16, NEXP])
    ones_f = k.sb("ones_f", [128, 128], F32); bof = Buf()
    V(lambda e: e.memset(ones_f[:], 1.0), [], [bof])
    epsr = k.sb("epsr", [128, 1], F32); bepsr = Buf()
    V(lambda e: e.memset(epsr[:], RMS_EPS), [], [bepsr])
    ln = LNCtx(k, ones_f, bof, gam, bgam, bet, bbet)

    s5f = k.sb("s5f", [128, 4, 512], F32); bs5f = Buf()
    s5b = k.sb("s5b", [128, 4, 512], BF16); bs5b = Buf()
    Y = k.sb("Y", [128, 16, 512], BF16); bY = bufs(16)
    of = k.sb("of", [128, 4, 512], F32); bof_ = Buf()
    gf = k.sb("gf", [128, 4, 512], F32); bgf = Buf()
    tA = [k.sb("tA", [128, 512], F32) for _ in range(2)]; btA = bufs(2)
    tB = [k.sb("tB", [128, 512], F32) for _ in range(2)]; btB = bufs(2)
    xc = [k.sb("xc", [128, 512], F32) for _ in range(3)]; bxc = bufs(3)
    z = k.sb("z", [128, 16, 512], F32); bz = bufs(16)
    x1f, bx1f = z, bz
    xb = [k.sb("xb", [128, 512], BF16) for _ in range(2)]; bxb = bufs(2)
    NPS = 3
    psg = [k.ps("psg", [128, 512]) for _ in range(NPS)]; bpsg = bufs(NPS)
    psc = [0]

    def nps():
        i = psc[0] % NPS
        psc[0] += 1
        return psg[i], bpsg[i]

    if with_router:
        psr = k.ps("psr", [128, 512]); bpsr = Buf()
        lg = k.sb("lg", [128, 8], F32); blg = Buf()
        m8 = k.sb("m8", [128, 8], F32); bm8 = Buf()
        nm1 = k.sb("nm1", [128, 1], F32); bnm1 = Buf()
        ee = k.sb("ee", [128, 8], F32); bee = Buf()
        sl = k.sb("sl", [128, 8], F32); bsl = Buf()
        den = k.sb("den", [128, 1], F32); bden = Buf()
        gt = [k.sb("gt", [128, 8], F32) for _ in range(2)]; bgt = bufs(2)

    for tt in range(NT):
        tsl = slice(tt * 512, (tt + 1) * 512)
        k.dma("sync", out=s5f[:], in_=ys5.rearrange("(kc p) t -> p kc t", p=128)[:, :, tsl], w=[bs5f])
        G(lambda e: e.tensor_copy(out=s5b[:], in_=s5f[:]), [bs5f], [bs5b])
        for mc in range(4):
            ps, bps = nps()
            for kc in range(4):
                PE(lambda e: e.matmul(ps[:], lhsT=wglu[:, kc, mc * 128:(mc + 1) * 128], rhs=s5b[:, kc, :],
                                      start=(kc == 0), stop=(kc == 3)), [bwglu, bs5b], [bps])
            j = mc % 2
            A(lambda e: e.activation(out=tA[j][:], in_=ps[:], func=AF.Sigmoid, bias=bglu[:, mc:mc + 1]),
              [bps, bbglu], [btA[j]])
            V(lambda e: e.tensor_tensor(out=Y[:, mc, :], in0=s5f[:, mc, :], in1=tA[j][:], op=ALU.mult),
              [bs5f, btA[j]], [bY[mc]])
        k.dma("gpsimd", out=Y[:, 4:12, :], in_=ymla.rearrange("(kc p) t -> p kc t", p=128)[:, :, tsl], w=bY[4:12])
        k.dma("sync", out=of[:], in_=ohg.rearrange("(kc p) t -> p kc t", p=128)[:, :, tsl], w=[bof_])
        k.dma("sync", out=gf[:], in_=hg.rearrange("(kc p) t -> p kc t", p=128)[:, :, tsl], w=[bgf])
        for hc in range(4):
            j = hc % 2
            A(lambda e: e.activation(out=tA[j][:], in_=of[:, hc, :], func=AF.Square), [bof_], [btA[j]])
            ps, bps = nps()
            PE(lambda e: e.matmul(ps[:], lhsT=ones_f[:], rhs=tA[j][:], start=True, stop=True), [bof, btA[j]], [bps])
            A(lambda e: e.activation(out=tB[j][:], in_=ps[:], func=AF.Sqrt, scale=1.0 / 128, bias=epsr[:, 0:1]),
              [bps, bepsr], [btB[j]])
            V(lambda e: e.reciprocal(out=tB[j][:], in_=tB[j][:]), [btB[j]], [btB[j]])
            V(lambda e: e.scalar_tensor_tensor(out=tB[j][:], in0=of[:, hc, :], scalar=onorm[:, 0:1], in1=tB[j][:],
                                               op0=ALU.mult, op1=ALU.mult), [bof_, bonorm, btB[j]], [btB[j]])
            A(lambda e: e.activation(out=tA[j][:], in_=gf[:, hc, :], func=AF.Silu), [bgf], [btA[j]])
            G(lambda e: e.tensor_tensor(out=Y[:, 12 + hc, :], in0=tB[j][:], in1=tA[j][:], op=ALU.mult),
              [btB[j], btA[j]], [bY[12 + hc]])
        for mc in range(16):
            xi_ = mc % 3
            k.dma("sync", out=xc[xi_][:], in_=xT[mc * 128:(mc + 1) * 128, tsl], w=[bxc[xi_]])
            ps, bps = nps()
            for kc in range(16):
                PE(lambda e: e.matmul(ps[:], lhsT=wout[:, kc, mc * 128:(mc + 1) * 128], rhs=Y[:, kc, :],
                                      start=(kc == 0), stop=(kc == 15)), [bwout[kc], bY[kc]], [bps])
            V(lambda e: e.scalar_tensor_tensor(out=z[:, mc, :], in0=xc[xi_][:], scalar=ALPHA, in1=ps[:],
                                               op0=ALU.mult, op1=ALU.add), [bxc[xi_], bps], [bz[mc]])

        def emit(mc, t, bt):
            G(lambda e: e.tensor_copy(out=x1f[:, mc, :], in_=t[:]), [bt], [bx1f[mc]])
            k.dma("sync", out=x1o[mc * 128:(mc + 1) * 128, tsl], in_=x1f[:, mc, :], r=[bx1f[mc]])
            j = mc % 2
            G(lambda e: e.tensor_copy(out=xb[j][:], in_=t[:]), [bt], [bxb[j]])
            k.dma("sync", out=x1bo[mc * 128:(mc + 1) * 128, tsl], in_=xb[j][:], r=[bxb[j]])

        ln.run(z, bz, emit)
        if with_router:
            for b4 in range(4):
                bs = slice(b4 * 128, (b4 + 1) * 128)
                for kc in range(16):
                    PE(lambda e: e.matmul(psr[:, 0:NEXP], lhsT=x1f[:, kc, bs], rhs=wr[:, kc, :], start=(kc == 0), stop=(kc == 15)),
                       [bx1f[kc], bwr], [bpsr])
                V(lambda e: e.tensor_copy(out=lg[:], in_=psr[:, 0:NEXP]), [bpsr], [blg])
                V(lambda e: e.max(out=m8[:], in_=lg[:]), [blg], [bm8])
                V(lambda e: e.tensor_scalar(out=nm1[:], in0=m8[:, 0:1], scalar1=-1.0, scalar2=None, op0=ALU.mult), [bm8], [bnm1])
                A(lambda e: e.activation(out=ee[:], in_=lg[:], func=AF.Exp, bias=nm1[:, 0:1]), [blg, bnm1], [bee])
                V(lambda e: e.tensor_scalar(out=sl[:], in0=lg[:], scalar1=m8[:, 1:2], scalar2=None, op0=ALU.is_ge), [blg, bm8], [bsl])
                V(lambda e: e.tensor_tensor(out=ee[:], in0=ee[:], in1=sl[:], op=ALU.mult), [bee, bsl], [bee])
                V(lambda e: e.reduce_sum(out=den[:], in_=ee[:], axis=AX.X), [bee], [bden])
                V(lambda e: e.reciprocal(out=den[:], in_=den[:]), [bden], [bden])
                gi = b4 % 2
                V(lambda e: e.tensor_scalar(out=gt[gi][:], in0=ee[:], scalar1=den[:, 0:1], scalar2=None, op0=ALU.mult),
                  [bee, bden], [bgt[gi]])
                r0 = tt * 512 + b4 * 128
                k.dma("sync", out=gout[r0:r0 + 128, :], in_=gt[gi][:], r=[bgt[gi]])
    return k.finish()


def c1_inputs(inp, layer, c, ys5T, ymlaT, ohgT, projT, xT, with_router):
    ts = slice(c * TOK, (c + 1) * TOK)
    d = {
        "ys5": np.ascontiguousarray(ys5T[:, ts]),
        "ymla": np.ascontiguousarray(ymlaT[:, ts]),
        "ohg": np.ascontiguousarray(ohgT[:, ts]),
        "hg": np.ascontiguousarray(projT[1344 + 1536:1344 + 2048, ts]),
        "xT": np.ascontiguousarray(xT[:, ts]),
        "wglu": inp["s5_w_glu"][layer],
        "bglu": np.ascontiguousarray(inp["s5_b_glu"][layer].reshape(4, 128).T),
        "onorm": np.ascontiguousarray(inp["hg_o_norm"][layer].reshape(128, 1)),
        "wout": inp["w_out"][layer],
        "lng": np.ascontiguousarray(inp["ln1_g"][layer].reshape(16, 128).T),
        "lnb": np.ascontiguousarray(inp["ln1_b"][layer].reshape(16, 128).T),
    }
    if with_router:
        d["wr"] = inp["moe_router"][layer // 2]
    return d


def build_M(F, with_gate):
    k = KB()
    ST = 1024
    NS = SEQ // ST
    xbf = k.din("xbf", [D, SEQ], BF16)
    d_wg = k.din("wg", [D, F])
    d_wu = k.din("wu", [D, F])
    d_wd = k.din("wd", [F, D])
    if with_gate:
        d_gate = k.din("gate", [128, SEQ])
    out = k.dout("part", [D, SEQ])

    def V(fn, r, w):
        return k.op("vector", fn, r=r, w=w)

    def G(fn, r, w):
        return k.op("gpsimd", fn, r=r, w=w)

    def A(fn, r, w):
        return k.op("scalar", fn, r=r, w=w)

    def PE(fn, r, w):
        return k.op("tensor", fn, r=r, w=w)

    blocks = []
    f0 = 0
    while f0 < F:
        bsz = min(256, F - f0)
        chunks = [(c0, min(128, bsz - c0)) for c0 in range(0, bsz, 128)]
        blocks.append((f0, bsz, chunks))
        f0 += bsz
    wg = [k.sb("wg", [128, 16, 256], BF16) for _ in range(2)]; bwg = bufs(2)
    wu = [k.sb("wu", [128, 16, 256], BF16) for _ in range(2)]; bwu = bufs(2)
    wd = [k.sb("wd", [128, 2, D], BF16) for _ in range(2)]; bwd = bufs(2)
    xs = k.sb("xs", [128, 16, ST], BF16); bxs = Buf()
    acc = k.sb("acc", [128, 16, ST], F32); bacc = [bufs(16), bufs(16)]
    if with_gate:
        gts = k.sb("gts", [128, ST], F32); bgts = Buf()
    sil = [k.sb("sil", [128, 512], F32) for _ in range(2)]; bsil = bufs(2)
    hh = [k.sb("hh", [128, 2, 512], BF16) for _ in range(2)]; bhh = [bufs(2), bufs(2)]
    psG = [k.ps("psG", [128, 512]) for _ in range(2)]; bpsG = bufs(2)
    psU = [k.ps("psU", [128, 512]) for _ in range(2)]; bpsU = bufs(2)
    psD = [k.ps("psD", [128, 512]) for _ in range(4)]; bpsD = bufs(4)
    xv = xbf.rearrange("(kc p) t -> p kc t", p=128)
    wi = 0
    gi = 0
    di = 0
    hi_ = 0
    for s in range(NS):
        ssl = slice(s * ST, (s + 1) * ST)
        k.dma("sync", out=xs[:], in_=xv[:, :, ssl], w=[bxs])
        if with_gate:
            k.dma("sync", out=gts[:], in_=d_gate[:, ssl], w=[bgts])
        for bi, (f0, bsz, chunks) in enumerate(blocks):
            w_ = wi % 2
            wi += 1
            k.dma("gpsimd", out=wg[w_][:, :, 0:bsz], in_=d_wg.rearrange("(kc p) f -> p kc f", p=128)[:, :, f0:f0 + bsz], w=[bwg[w_]])
            k.dma("gpsimd", out=wu[w_][:, :, 0:bsz], in_=d_wu.rearrange("(kc p) f -> p kc f", p=128)[:, :, f0:f0 + bsz], w=[bwu[w_]])
            for ci, (c0, csz) in enumerate(chunks):
                k.dma("gpsimd", out=wd[w_][0:csz, ci, :], in_=d_wd[f0 + c0:f0 + c0 + csz, :], w=[bwd[w_]])
            for sub in range(ST // 512):
                tsl = slice(sub * 512, (sub + 1) * 512)
                h_ = hi_ % 2
                hi_ += 1
                for ci, (c0, csz) in enumerate(chunks):
                    g_ = gi % 2
                    gi += 1
                    for kc in range(16):
                        PE(lambda e: e.matmul(psG[g_][0:csz, :], lhsT=wg[w_][:, kc, c0:c0 + csz], rhs=xs[:, kc, tsl],
                                              start=(kc == 0), stop=(kc == 15)), [bwg[w_], bxs], [bpsG[g_]])
                    for kc in range(16):
                        PE(lambda e: e.matmul(psU[g_][0:csz, :], lhsT=wu[w_][:, kc, c0:c0 + csz], rhs=xs[:, kc, tsl],
                                              start=(kc == 0), stop=(kc == 15)), [bwu[w_], bxs], [bpsU[g_]])
                    A(lambda e: e.activation(out=sil[g_][0:csz, :], in_=psG[g_][0:csz, :], func=AF.Silu), [bpsG[g_]], [bsil[g_]])
                    if with_gate:
                        V(lambda e: e.tensor_tensor(out=sil[g_][0:csz, :], in0=sil[g_][0:csz, :], in1=psU[g_][0:csz, :], op=ALU.mult),
                          [bsil[g_], bpsU[g_]], [bsil[g_]])
                        G(lambda e: e.tensor_tensor(out=hh[h_][0:csz, ci, :], in0=sil[g_][0:csz, :], in1=gts[0:csz, tsl], op=ALU.mult),
                          [bsil[g_], bgts], [bhh[h_][ci]])
                    else:
                        V(lambda e: e.tensor_tensor(out=hh[h_][0:csz, ci, :], in0=sil[g_][0:csz, :], in1=psU[g_][0:csz, :], op=ALU.mult),
                          [bsil[g_], bpsU[g_]], [bhh[h_][ci]])
                for fc in range(16):
                    d_ = di % 4
                    di += 1
                    for ci, (c0, csz) in enumerate(chunks):
                        PE(lambda e: e.matmul(psD[d_][:], lhsT=wd[w_][0:csz, ci, fc * 128:(fc + 1) * 128], rhs=hh[h_][0:csz, ci, :],
                                              start=(ci == 0), stop=(ci == len(chunks) - 1)), [bwd[w_], bhh[h_][ci]], [bpsD[d_]])
                    if bi == 0:
                        V(lambda e: e.tensor_copy(out=acc[:, fc, tsl], in_=psD[d_][:]), [bpsD[d_]], [bacc[sub][fc]])
                    else:
                        V(lambda e: e.tensor_tensor(out=acc[:, fc, tsl], in0=psD[d_][:], in1=acc[:, fc, tsl], op=ALU.add),
                          [bpsD[d_], bacc[sub][fc]], [bacc[sub][fc]])
        for fc in range(16):
            k.dma("sync", out=out[fc * 128:(fc + 1) * 128, ssl], in_=acc[:, fc, :], r=[bacc[0][fc], bacc[1][fc]])
    return k.finish()


def build_C2():
    k = KB()
    NT = TOK // 512
    parts = k.din("parts", [NEXP, D, TOK])
    x1 = k.din("x1T", [D, TOK])
    d_g = k.din("lng", [128, 16])
    d_b = k.din("lnb", [128, 16])
    out = k.dout("x2T", [D, TOK])

    def V(fn, r, w):
        return k.op("vector", fn, r=r, w=w)

    def G(fn, r, w):
        return k.op("gpsimd", fn, r=r, w=w)

    gam = k.sb("gam", [128, 16], F32); bgam = Buf()
    k.dma("sync", out=gam[:], in_=d_g, w=[bgam])
    bet = k.sb("bet", [128, 16], F32); bbet = Buf()
    k.dma("sync", out=bet[:], in_=d_b, w=[bbet])
    ones_f = k.sb("ones_f", [128, 128], F32); bof = Buf()
    V(lambda e: e.memset(ones_f[:], 1.0), [], [bof])
    ln = LNCtx(k, ones_f, bof, gam, bgam, bet, bbet)
    z = k.sb("z", [128, 16, 512], F32); bz = bufs(16)
    pin = [k.sb("pin", [128, NEXP, 512], F32) for _ in range(2)]; bpin = bufs(2)
    xin = [k.sb("xin", [128, 512], F32) for _ in range(2)]; bxin = bufs(2)
    ob = [k.sb("ob", [128, 512], F32) for _ in range(3)]; bob = bufs(3)
    oc = [0]
    for tt in range(NT):
        tsl = slice(tt * 512, (tt + 1) * 512)
        for mc in range(16):
            j = mc % 2
            k.dma("sync", out=pin[j][:], in_=parts[:, mc * 128:(mc + 1) * 128, tsl].rearrange("e p t -> p e t"), w=[bpin[j]])
            k.dma("sync", out=xin[j][:], in_=x1[mc * 128:(mc + 1) * 128, tsl], w=[bxin[j]])
            eng = V if mc % 2 == 0 else G
            eng(lambda e: e.scalar_tensor_tensor(out=z[:, mc, :], in0=xin[j][:], scalar=ALPHA, in1=pin[j][:, 0, :],
                                                 op0=ALU.mult, op1=ALU.add) if eng is V else
                e.tensor_scalar(out=z[:, mc, :], in0=xin[j][:], scalar1=ALPHA, scalar2=None, op0=ALU.mult),
                [bxin[j], bpin[j]], [bz[mc]])
            for e_ in range(0 if eng is G else 1, NEXP):
                eng(lambda e: e.tensor_tensor(out=z[:, mc, :], in0=z[:, mc, :], in1=pin[j][:, e_, :], op=ALU.add),
                    [bz[mc], bpin[j]], [bz[mc]])

        def emit(mc, t, bt):
            o = oc[0] % 3
            oc[0] += 1
            G(lambda e: e.tensor_copy(out=ob[o][:], in_=t[:]), [bt], [bob[o]])
            k.dma("sync", out=out[mc * 128:(mc + 1) * 128, tsl], in_=ob[o][:], r=[bob[o]])

        ln.run(z, bz, emit)
    return k.finish()


def kernel(**inp):
    inp = {k_: np.asarray(v) for k_, v in inp.items()}
    xT = np.ascontiguousarray(inp["x"][0].T)
    rope = rope_tables()
    mla_c = (rope[0], rope[1], causal_masks())
    hg_c = hg_consts()
    pA = get_prog("A", build_A)
    pS5 = get_prog("S5", build_S5)
    pMLA = get_prog("MLA", build_MLA)
    pHG = get_prog("HG", build_HG)
    pC2 = get_prog("C2", build_C2)
    for layer in range(2):
        moe = (layer % 2 == 1)
        res = run(pA, [{"xT": np.ascontiguousarray(xT[:, c * TOK:(c + 1) * TOK]), "w": inp["w_in"][layer]} for c in range(NCORES)])
        projT = np.concatenate([r["projT"] for r in res], axis=1)
        res = run(pS5, [s5_inputs(inp, layer, projT, c) for c in range(NCORES)])
        ys5T = np.concatenate([r["ys5"] for r in res], axis=0)
        res = run(pMLA, [mla_inputs(inp, layer, projT, c, mla_c) for c in range(NCORES)])
        ymlaT = np.ascontiguousarray(np.concatenate([r["y"] for r in res], axis=1).T)
        res = run(pHG, [hg_inputs(inp, layer, projT, c, hg_c) for c in range(NCORES)])
        ohgT = np.concatenate([r["ohg"] for r in res], axis=0)
        pC1 = get_prog("C1r" if moe else "C1", lambda: build_C1(moe))
        res = run(pC1, [c1_inputs(inp, layer, c, ys5T, ymlaT, ohgT, projT, xT, moe) for c in range(NCORES)])
        x1T = np.concatenate([r["x1T"] for r in res], axis=1)
        x1bf = np.concatenate([r["x1bf"] for r in res], axis=1)
        j = layer // 2
        if moe:
            gates = np.concatenate([r["gates"] for r in res], axis=0)
            pM = get_prog("Mmoe", lambda: build_M(D_FFE, True))
            in_maps = [{"xbf": x1bf, "wg": inp["moe_w_gate"][j][e], "wu": inp["moe_w_up"][j][e], "wd": inp["moe_w_down"][j][e],
                        "gate": np.ascontiguousarray(np.broadcast_to(gates[:, e][None, :], (128, SEQ)))} for e in range(NCORES)]
        else:
            Fs = D_FF // NCORES
            pM = get_prog("Mdense", lambda: build_M(Fs, False))
            in_maps = [{"xbf": x1bf, "wg": np.ascontiguousarray(inp["ffn_w_gate"][j][:, c * Fs:(c + 1) * Fs]),
                        "wu": np.ascontiguousarray(inp["ffn_w_up"][j][:, c * Fs:(c + 1) * Fs]),
                        "wd": np.ascontiguousarray(inp["ffn_w_down"][j][c * Fs:(c + 1) * Fs, :])} for c in range(NCORES)]
        res = run(pM, in_maps)
        parts = [r["part"] for r in res]
        lng = np.ascontiguousarray(inp["ln2_g"][layer].reshape(16, 128).T)
        lnb = np.ascontiguousarray(inp["ln2_b"][layer].reshape(16, 128).T)
        in_maps = [{"parts": np.ascontiguousarray(np.stack([p[:, c * TOK:(c + 1) * TOK] for p in parts], axis=0)),
                    "x1T": np.ascontiguousarray(x1T[:, c * TOK:(c + 1) * TOK]), "lng": lng, "lnb": lnb} for c in range(NCORES)]
        del parts
        res = run(pC2, in_maps)
        xT = np.concatenate([r["x2T"] for r in res], axis=1)
    return np.ascontiguousarray(xT.T)[None].astype(np.float32)
```
